# Optimizing a Trainium2 kernel written in Bass

```python
import jax, jax.numpy as jnp
from jax import lax
import numpy as np

D_MODEL = 1024
BATCH = 4
SEQ = 8192
DEPTH = 4

MLSTM_HEADS = 4
MLSTM_HEAD_DIM = D_MODEL // MLSTM_HEADS
MLSTM_WIDTH = MLSTM_HEADS * MLSTM_HEAD_DIM
MLSTM_CHUNK = 64
QK_CONV_WIDTH = 4
CONV_WIDTH = D_MODEL
SHORT_CONV_K = 3
D_FF = 3584
N_EXPERTS = 8
TOP_K = 2
N_DENSE = (DEPTH + 1) // 2
N_MOE = DEPTH // 2
ALPHA = (2 * DEPTH) ** 0.25
BETA = (8 * DEPTH) ** -0.25
LN_EPS = 1e-5
SPLIT_SIZES = (2 * MLSTM_WIDTH, MLSTM_WIDTH, MLSTM_WIDTH, MLSTM_HEADS, MLSTM_HEADS,
               CONV_WIDTH, CONV_WIDTH, CONV_WIDTH, D_MODEL, D_MODEL)
PROJ_WIDTH = sum(SPLIT_SIZES)

kernel_name = 'hybrid_mlstm_shortconv_moe_deepnorm'


def _split_points():
    pts, acc = [], 0
    for s in SPLIT_SIZES[:-1]:
        acc += s
        pts.append(acc)
    return pts


def layer_norm(x, g, b):
    xf = x.astype(jnp.float32)
    mu = xf.mean(-1, keepdims=True)
    var = jnp.square(xf - mu).mean(-1, keepdims=True)
    y = (xf - mu) * lax.rsqrt(var + LN_EPS) * g.astype(jnp.float32) + b.astype(jnp.float32)
    return y.astype(x.dtype)


def causal_depthwise_conv(u, w):
    K = w.shape[0]
    return lax.conv_general_dilated(
        u, w[:, None, :].astype(u.dtype), window_strides=(1,), padding=[(K - 1, 0)],
        dimension_numbers=('NWC', 'WIO', 'NWC'), feature_group_count=u.shape[-1])


def mlstm_chunkwise(q, k, v, i_pre, f_pre):
    Bsz, H, S, d = q.shape
    L = MLSTM_CHUNK
    nc = S // L
    f32 = jnp.float32
    lf = jax.nn.log_sigmoid(f_pre.astype(f32))
    li = i_pre.astype(f32)

    def chunks(t):
        t = t.astype(f32).reshape((Bsz, H, nc, L) + t.shape[3:])
        return jnp.moveaxis(t, 2, 0)

    qc, kc, vc, lic = chunks(q), chunks(k), chunks(v), chunks(li)
    bc = jnp.cumsum(chunks(lf), axis=-1)
    mask = jnp.tril(jnp.ones((L, L), dtype=bool))

    def step(carry, xs):
        C, n, m = carry
        qj, kj, vj, bj, lij = xs
        a = bj + m[..., None]
        Dm = bj[..., :, None] - bj[..., None, :] + lij[..., None, :]
        Dm = jnp.where(mask, Dm, -jnp.inf)
        mt = jnp.maximum(a, Dm.max(-1))
        W = jnp.exp(Dm - mt[..., None])
        scores = jnp.einsum('bhtk,bhsk->bhts', qj, kj) * W
        inter = jnp.exp(a - mt)
        num = jnp.einsum('bhts,bhsv->bhtv', scores, vj) \
            + inter[..., None] * jnp.einsum('bhtk,bhkv->bhtv', qj, C)
        den = scores.sum(-1) + inter * jnp.einsum('bhtk,bhk->bht', qj, n)
        h = num / jnp.maximum(jnp.abs(den), jnp.exp(-mt))[..., None]
        bL = bj[..., -1]
        g = bL[..., None] - bj + lij
        m_new = jnp.maximum(bL + m, g.max(-1))
        decay = jnp.exp(bL + m - m_new)
        ws = jnp.exp(g - m_new[..., None])
        kw = kj * ws[..., None]
        C_new = decay[..., None, None] * C + jnp.einsum('bhsk,bhsv->bhkv', kw, vj)
        n_new = decay[..., None] * n + kw.sum(-2)
        return (C_new, n_new, m_new), h

    init = (jnp.zeros((Bsz, H, d, d), f32), jnp.zeros((Bsz, H, d), f32), jnp.zeros((Bsz, H), f32))
    _, hs = lax.scan(step, init, (qc, kc, vc, bc, lic))
    return jnp.moveaxis(hs, 0, 2).reshape(Bsz, H, S, d)


def token_mixer(h, w_in, b_gates, conv_qk, hn_gain, conv_short, w_out):
    Bsz, S, _ = h.shape
    H, Dh, W = MLSTM_HEADS, MLSTM_HEAD_DIM, MLSTM_WIDTH
    p = h @ w_in
    qk, v, o, ig, fg, cb, cc, cx, ga, gb = jnp.split(p, _split_points(), axis=-1)
    qk = jax.nn.silu(causal_depthwise_conv(qk, conv_qk))
    q, k = jnp.split(qk, 2, axis=-1)
    heads = lambda t: t.reshape(Bsz, S, H, Dh).transpose(0, 2, 1, 3)
    i_pre = (ig + b_gates[:H]).transpose(0, 2, 1)
    f_pre = (fg + b_gates[H:]).transpose(0, 2, 1)
    hm = mlstm_chunkwise(heads(q), heads(k) * (Dh ** -0.5), heads(v), i_pre, f_pre)
    hm = hm.transpose(0, 2, 1, 3)
    mu = hm.mean(-1, keepdims=True)
    var = jnp.square(hm - mu).mean(-1, keepdims=True)
    hm = (hm - mu) * lax.rsqrt(var + LN_EPS) * hn_gain.reshape(H, Dh).astype(jnp.float32)
    y_a = jax.nn.sigmoid(o) * hm.reshape(Bsz, S, W).astype(h.dtype)
    y_b = cb * causal_depthwise_conv(cc * cx, conv_short)
    merged = jax.nn.sigmoid(ga) * y_a + jax.nn.sigmoid(gb) * y_b
    return merged @ w_out


def swiglu(h, w13, w2):
    a, b = jnp.split(h @ w13, 2, axis=-1)
    return (jax.nn.silu(a) * b) @ w2


def moe_ffn(h, w_router, w13, w2):
    Bsz, S, D = h.shape
    t = h.reshape(Bsz * S, D)
    logits = (t @ w_router).astype(jnp.float32)
    top_v, top_i = lax.top_k(logits, TOP_K)
    probs = jax.nn.softmax(top_v, axis=-1)
    gates = (jax.nn.one_hot(top_i, N_EXPERTS, dtype=jnp.float32) * probs[..., None]).sum(-2)
    out = jnp.zeros_like(t)
    for e in range(N_EXPERTS):
        out = out + gates[:, e, None].astype(t.dtype) * swiglu(t, w13[e], w2[e])
    return out.reshape(Bsz, S, D)


def setup_inputs(seed: int = 0) -> dict:
    key = jax.random.key(seed)
    ks = jax.random.split(key, 20)
    f32 = jnp.float32
    nrm = lambda k, shape, scale: jax.random.normal(k, shape, f32) * scale
    D, H, W, Wc = D_MODEL, MLSTM_HEADS, MLSTM_WIDTH, CONV_WIDTH
    b_gates = jnp.concatenate(
        [nrm(ks[5], (DEPTH, H), 0.1),
         jnp.linspace(3.0, 6.0, H, dtype=f32)[None, :] + nrm(ks[6], (DEPTH, H), 0.1)], axis=-1)
    return {
        'x': nrm(ks[0], (BATCH, SEQ, D), 1.0),
        'c': nrm(ks[1], (BATCH, D), 1.0),
        'w_ada': nrm(ks[2], (DEPTH, D, 6 * D), 0.5 * D ** -0.5),
        'b_ada': nrm(ks[3], (DEPTH, 6 * D), 0.02),
        'w_in': nrm(ks[4], (DEPTH, D, PROJ_WIDTH), D ** -0.5),
        'b_gates': b_gates,
        'conv_qk': nrm(ks[7], (DEPTH, QK_CONV_WIDTH, 2 * W), QK_CONV_WIDTH ** -0.5),
        'hn_gain': 1.0 + nrm(ks[8], (DEPTH, W), 0.05),
        'conv_short': nrm(ks[9], (DEPTH, SHORT_CONV_K, Wc), SHORT_CONV_K ** -0.5),
        'w_out': nrm(ks[10], (DEPTH, D, D), BETA * D ** -0.5),
        'ln_g': 1.0 + nrm(ks[11], (DEPTH, 2, D), 0.05),
        'ln_b': nrm(ks[12], (DEPTH, 2, D), 0.02),
        'dense_w13': nrm(ks[13], (N_DENSE, D, 2 * D_FF), D ** -0.5),
        'dense_w2': nrm(ks[14], (N_DENSE, D_FF, D), BETA * D_FF ** -0.5),
        'w_router': nrm(ks[15], (N_MOE, D, N_EXPERTS), D ** -0.5),
        'moe_w13': nrm(ks[16], (N_MOE, N_EXPERTS, D, 2 * D_FF), D ** -0.5),
        'moe_w2': nrm(ks[17], (N_MOE, N_EXPERTS, D_FF, D), BETA * D_FF ** -0.5),
    }


def reference(x, c, w_ada, b_ada, w_in, b_gates, conv_qk, hn_gain, conv_short, w_out,
              ln_g, ln_b, dense_w13, dense_w2, w_router, moe_w13, moe_w2):
    cond = jax.nn.silu(c)
    for l in range(DEPTH):
        mod = (cond @ w_ada[l] + b_ada[l])[:, None, :]
        sh_a, sc_a, g_a, sh_f, sc_f, g_f = jnp.split(mod, 6, axis=-1)
        h = x * (1 + sc_a) + sh_a
        y = token_mixer(h, w_in[l], b_gates[l], conv_qk[l], hn_gain[l], conv_short[l], w_out[l])
        x = layer_norm(ALPHA * x + (1 + g_a) * y, ln_g[l, 0], ln_b[l, 0])
        h = x * (1 + sc_f) + sh_f
        if l % 2 == 0:
            y = swiglu(h, dense_w13[l // 2], dense_w2[l // 2])
        else:
            y = moe_ffn(h, w_router[l // 2], moe_w13[l // 2], moe_w2[l // 2])
        x = layer_norm(ALPHA * x + (1 + g_f) * y, ln_g[l, 1], ln_b[l, 1])
    return x
```

```python
import contextlib
import numpy as np
import concourse.bass as bass
import concourse.mybir as mybir
from concourse.bass_utils import run_bass_kernel_spmd

F32 = mybir.dt.float32
BF16 = mybir.dt.bfloat16
U8 = mybir.dt.uint8
AF = mybir.ActivationFunctionType
ALU = mybir.AluOpType

D = 1024
H = 4
DH = 256
PW = 9224
DFF = 3584
NE = 8
NF = DFF // 128
LN_EPS = 1e-5
T = 512
NSUB = 4

import os
_DBG = os.environ.get("FFN_DBG", "")
COMPUTE = ("pe", "act", "dve", "pool")
QUEUES = ("sp", "gq")


class Buf:
    __slots__ = ("name", "writer", "readers")

    def __init__(self, name):
        self.name = name
        self.writer = None
        self.readers = []


class Op:
    __slots__ = ("eng", "fn", "deps", "is_dma", "signal", "count", "sem", "stream")

    def __init__(self, eng, fn, is_dma, stream):
        self.eng = eng
        self.fn = fn
        self.deps = []
        self.is_dma = is_dma
        self.signal = False
        self.count = 0
        self.sem = None
        self.stream = stream


class Prog:
    def __init__(self, nc, n_dma_sems=12):
        self.nc = nc
        self.ops = {e: [] for e in COMPUTE + ("sp",)}
        self.n_dma_sems = n_dma_sems
        self.all_ops = []
        self.bar = []
        self.need_bar = set()
        self.dmas_since_bar = []

    def op(self, eng, fn, reads=(), writes=()):
        is_dma = eng in QUEUES
        stream = "pool" if eng == "gq" else eng
        o = Op(eng, fn, is_dma, stream)
        deps = []
        pe = stream == "pe"
        for b in reads:
            w = b.writer
            if w is not None and (w.is_dma or w.stream != stream or not pe):
                deps.append(w)
        for b in writes:
            w = b.writer
            if w is not None and (w.is_dma or w.stream != stream or not pe):
                deps.append(w)
            for r in b.readers:
                if r.is_dma or r.stream != stream or not pe:
                    deps.append(r)
        if stream in self.need_bar:
            deps.extend(self.bar)
            self.need_bar.discard(stream)
        o.deps = deps
        for b in reads:
            b.readers.append(o)
        for b in writes:
            b.writer = o
            b.readers = []
        self.all_ops.append(o)
        self.ops[stream].append(o)
        if is_dma:
            self.dmas_since_bar.append(o)
        return o

    def barrier(self):
        bar = list(self.dmas_since_bar)
        for s, lst in self.ops.items():
            for o in reversed(lst):
                if not o.is_dma:
                    bar.append(o)
                    break
        self.bar = bar
        self.need_bar = set(self.ops.keys())
        self.dmas_since_bar = []

    def emit(self, final_wait_ops=()):
        nc = self.nc
        streams = list(self.ops.keys())
        for o in self.all_ops:
            for d in o.deps:
                d.signal = True
        for o in final_wait_ops:
            o.signal = True
        with contextlib.ExitStack() as es:
            esem = {s: es.enter_context(nc.semaphore("s_" + s)) for s in streams}
            dsem = {q: [es.enter_context(nc.semaphore(f"d_{q}{i}")) for i in range(self.n_dma_sems)]
                    for q in QUEUES}
            ecount = {s: 0 for s in streams}
            dcount = {q: [0] * self.n_dma_sems for q in QUEUES}
            drr = {q: 0 for q in QUEUES}
            dprev = {q: [None] * self.n_dma_sems for q in QUEUES}
            for o in self.all_ops:
                if o.is_dma:
                    q = o.eng
                    i = drr[q]
                    drr[q] = (i + 1) % self.n_dma_sems
                    prev = dprev[q][i]
                    if prev is not None:
                        o.deps.append(prev)
                    dcount[q][i] += 16
                    o.sem = dsem[q][i]
                    o.count = dcount[q][i]
                    dprev[q][i] = o
                    o.signal = True
                elif o.signal:
                    ecount[o.stream] += 1
                    o.sem = esem[o.stream]
                    o.count = ecount[o.stream]
            self.n_waits = 0
            blk = es.enter_context(nc.Block())

            def run_stream(stream):
                def body(e):
                    waited = {}
                    for o in self.ops[stream]:
                        need = {}
                        for d in o.deps:
                            k = d.sem.name
                            if need.get(k, (None, 0))[1] < d.count:
                                need[k] = (d.sem, d.count)
                        for k, (sem, cnt) in need.items():
                            if waited.get(k, 0) < cnt:
                                e.wait_ge(sem, cnt)
                                waited[k] = cnt
                                self.n_waits += 1
                        ins = o.fn(e)
                        if o.signal:
                            ins.then_inc(o.sem, 16 if o.is_dma else 1)
                    if stream == "sp":
                        for o in final_wait_ops:
                            if waited.get(o.sem.name, 0) < o.count:
                                e.wait_ge(o.sem, o.count)
                                waited[o.sem.name] = o.count
                return body

            blk.tensor(run_stream("pe"))
            blk.scalar(run_stream("act"))
            blk.vector(run_stream("dve"))
            blk.gpsimd(run_stream("pool"))
            blk.sync(run_stream("sp"))


def _dtsize(dt):
    return {F32: 4, BF16: 2, U8: 1}[dt]


class Arena:
    def __init__(self, ap_u8, nbytes):
        self.base = ap_u8
        self.nbytes = nbytes
        self.off = 0

    def reset(self):
        self.off = 0

    def t(self, shape, dt):
        n = 1
        for s in shape[1:]:
            n *= s
        nb = n * _dtsize(dt)
        off = (self.off + 63) // 64 * 64
        assert off + nb <= self.nbytes, f"arena overflow: need {off + nb} > {self.nbytes}"
        self.off = off + nb
        v = self.base[:, off:off + nb].bitcast(dt)
        if len(shape) == 3:
            v = v.rearrange("p (a b) -> p a b", a=shape[1])
        elif len(shape) == 4:
            v = v.rearrange("p (a b c) -> p a b c", a=shape[1], b=shape[2])
        if shape[0] != 128:
            v = v[0:shape[0]]
        return v


class K:
    def __init__(self, NT):
        self.NT = NT
        self.NTILE = NT // T
        self.nc = bass.Bass("TRN2", target_bir_lowering=False)
        self.P = Prog(self.nc)
        self.es = contextlib.ExitStack()
        self.outs = []
        self.deferred = []
        self.pump_rate = 0
        nc = self.nc
        es = self.es
        self.ident = es.enter_context(nc.sbuf_tensor("ident", [128, 128], F32))
        self.identb = es.enter_context(nc.sbuf_tensor("identb", [128, 128], BF16))
        self.triu = es.enter_context(nc.sbuf_tensor("triu", [128, 128], F32))
        self.ones = es.enter_context(nc.sbuf_tensor("ones", [128, 128], F32))
        self.onesb = es.enter_context(nc.sbuf_tensor("onesb", [128, 2], BF16))
        self.epsc = es.enter_context(nc.sbuf_tensor("epsc", [128, 1], F32))
        self.flag = es.enter_context(nc.sbuf_tensor("flag_sb", [128, 1], F32))
        self.nl16 = es.enter_context(nc.sbuf_tensor("nl16", [128, 1], F32))
        self.b_const = Buf("const")
        self.b_flag = Buf("flag")
        self.b_modd = Buf("modd")
        self.b_xfer = Buf("xfer")
        ARENA = 196 * 1024
        self.arena_t = es.enter_context(nc.sbuf_tensor("arena", [128, ARENA], U8))
        self.A = Arena(self.arena_t, ARENA)
        self.psum = [es.enter_context(nc.psum_tensor(f"ps{i}", [128, 1024], F32)) for i in range(4)]
        self.pbuf = [[Buf(f"ps{i}_{h}") for h in range(2)] for i in range(4)]
        self.b_PA1 = [self.pbuf[0][1], self.pbuf[0][1]]
        P = self.P
        cb = [self.b_const]
        P.op("pool", lambda e: e.memset(self.ident[:], 0.0), writes=cb)
        P.op("pool", lambda e: e.affine_select(out=self.ident[:], in_=self.ident[:], pattern=[[-1, 128]],
                                               compare_op=ALU.not_equal, fill=1.0, base=0, channel_multiplier=1),
             reads=cb, writes=cb)
        P.op("pool", lambda e: e.tensor_copy(self.identb[:], self.ident[:]), reads=cb, writes=cb)
        P.op("pool", lambda e: e.memset(self.ones[:], 1.0), writes=cb)
        P.op("pool", lambda e: e.affine_select(out=self.triu[:], in_=self.ones[:], pattern=[[1, 128]],
                                               compare_op=ALU.is_ge, fill=0.0, base=0, channel_multiplier=-1),
             reads=cb, writes=cb)
        P.op("pool", lambda e: e.memset(self.onesb[:], 1.0), writes=cb)
        P.op("pool", lambda e: e.memset(self.epsc[:], LN_EPS), writes=cb)
        P.op("pool", lambda e: e.memset(self.nl16[:], -float(np.log(16.0))), writes=cb)

    def din(self, name, shape, dt=F32):
        return self.nc.dram_tensor(name, list(shape), dt, kind="ExternalInput").ap()

    def dout(self, name, shape, dt=F32):
        return self.nc.dram_tensor(name, list(shape), dt, kind="ExternalOutput").ap()

    def dint(self, name, shape, dt=F32):
        return self.nc.dram_tensor(name, list(shape), dt, kind="Internal").ap()

    def mm(self, out, lhsT, rhs, start, stop, reads, writes):
        return self.P.op("pe", lambda e: e.matmul(out, lhsT=lhsT, rhs=rhs, start=start, stop=stop),
                         reads=reads, writes=writes)

    def tr(self, out, in_, ident, reads, writes):
        return self.P.op("pe", lambda e: e.transpose(out, in_, ident), reads=reads, writes=writes)

    def act(self, out, in_, func, reads, writes, bias=None, scale=None):
        kw = {}
        if bias is not None:
            kw["bias"] = bias
        if scale is not None:
            kw["scale"] = scale
        return self.P.op("act", lambda e: e.activation(out, in_, func, **kw), reads=reads, writes=writes)

    def dve(self, fn, reads, writes):
        return self.P.op("dve", fn, reads=reads, writes=writes)

    def pool(self, fn, reads, writes):
        return self.P.op("pool", fn, reads=reads, writes=writes)

    def dma(self, q, out, in_, reads=(), writes=()):
        return self.P.op(q, lambda e: e.dma_start(out=out, in_=in_), reads=reads, writes=writes)

    def defer(self, q, out, in_, writes):
        self.deferred.append((q, out, in_, writes))

    def pump(self, n):
        for _ in range(min(n, len(self.deferred))):
            q, out, in_, writes = self.deferred.pop(0)
            self.dma(q, out, in_, writes=writes)

    def flush(self):
        self.pump(len(self.deferred))

    def load_flag(self, flag_d):
        self.dma("sp", self.flag[:], flag_d, writes=[self.b_flag])

    def finish(self):
        self.P.emit(final_wait_ops=self.outs)
        self.es.close()
        return self.nc

    def rows_to_cols(self, rows, R, cols, b_rows, b_cols, pbank, pb):
        for c in range(8):
            self.tr(pbank[:, c * R:(c + 1) * R], rows[0:R, c * 128:(c + 1) * 128], self.ident[0:R, 0:R],
                    reads=[b_rows, self.b_const], writes=[pb])
        self.dve(lambda e: e.tensor_copy(cols, pbank[:, 0:8 * R].rearrange("p (c r) -> p c r", c=8)),
                 reads=[pb], writes=[b_cols])


def phase_p0(k, c_d, w_ada_d, b_ada_d, modd, nl):
    A = k.A
    A.reset()
    crow = A.t([8, 128], F32)
    condT = A.t([128, 8], F32)
    wa = [A.t([128, 8, 512], F32) for _ in range(2)]
    brow = A.t([1, 6 * D], F32)
    mrow = A.t([1, 6 * D], F32)
    b_crow, b_cond, b_brow, b_mrow = Buf("crow"), Buf("condT"), Buf("brow"), Buf("mrow")
    b_wa = [Buf("wa0"), Buf("wa1")]
    ps = k.psum[0]
    pb = k.pbuf[0]
    k.dma("sp", crow, c_d[0].rearrange("(k p) -> k p", p=128), writes=[b_crow])
    k.tr(ps[:, 0:8], crow[0:8, 0:128], k.ident[0:8, 0:8], reads=[b_crow, k.b_const], writes=[pb[0]])
    k.act(condT, ps[:, 0:8], AF.Silu, reads=[pb[0]], writes=[b_cond])
    n = 0
    for l in range(nl):
        k.dma("sp", brow, b_ada_d[l:l + 1, :], writes=[b_brow])
        for j in range(12):
            s = n % 2
            n += 1
            k.dma("sp", wa[s], w_ada_d[l][:, j * 512:(j + 1) * 512].rearrange("(k p) n -> p k n", p=128),
                  writes=[b_wa[s]])
            pbank = ps[0:1, s * 512:(s + 1) * 512]
            for kk in range(8):
                k.mm(pbank, condT[:, kk:kk + 1], wa[s][:, kk, :], kk == 0, kk == 7,
                     reads=[b_cond, b_wa[s]], writes=[pb[s]])
            k.dve(lambda e, j=j, pbank=pbank: e.tensor_tensor(mrow[0:1, j * 512:(j + 1) * 512], pbank,
                                                              brow[0:1, j * 512:(j + 1) * 512], op=ALU.add),
                  reads=[pb[s], b_brow], writes=[b_mrow])
        o = k.dma("sp", modd[l:l + 1, :], mrow, reads=[b_mrow], writes=[k.b_modd])
        k.outs.append(o)
    k.P.barrier()


XF = 2048 + 8 + 48 + 16
ALPHA = 8.0 ** 0.25


def conv_mixer_weights(k, tag, w_in_l, w_out_l):
    W = {}
    W["win"] = k.dint(f"winb_{tag}", [20, 128, 4096], BF16)
    W["wg"] = k.dint(f"wgb_{tag}", [128, 64], BF16)
    W["wout"] = k.dint(f"woutb_{tag}", [128, 8192], BF16)
    W["b_win"] = [Buf(f"winb{i}") for i in range(20)]
    W["b_wg"] = Buf("wgb")
    W["b_wout"] = Buf("woutb")

    def srcv(c0, n):
        return w_in_l[:, c0:c0 + n].rearrange("(k p) n -> p k n", p=128)

    def dstv(blk):
        return W["win"][blk].rearrange("p (k n) -> p k n", k=8)

    for blk in range(8):
        k.defer("gq", dstv(blk), srcv(blk * 512, 512), writes=[W["b_win"][blk]])
    k.defer("gq", W["wg"].rearrange("p (k n) -> p k n", k=8), srcv(4096, 8), writes=[W["b_wg"]])
    for j in range(8):
        for i, base in enumerate((4104, 5128, 6152)):
            k.defer("gq", dstv(8 + j)[:, :, i * 128:(i + 1) * 128], srcv(base + j * 128, 128),
                  writes=[W["b_win"][8 + j]])
    for j in range(4):
        k.defer("gq", dstv(16 + j), srcv(7176 + j * 512, 512), writes=[W["b_win"][16 + j]])
    k.defer("gq", W["wout"].rearrange("p (k n) -> p k n", k=8),
          w_out_l.rearrange("(k p) n -> p k n", p=128), writes=[W["b_wout"]])
    return W


def phase_mixer(k, x_src, x_dst, mod_l, prm, W, xfer_in, xfer_out):
    A = k.A
    A.reset()
    P = k.P
    PA, PB, PC, PD = k.psum
    bPA, bPB, bPC, bPD = k.pbuf
    bPAh = [[bPA[0]], [bPA[1]]]
    xt = A.t([128, NSUB, D], F32)
    b_xt = Buf("xt")
    hT = A.t([128, 8, T], BF16)
    b_hT = [Buf(f"hT{c}") for c in range(8)]
    wsl = [A.t([128, 8, 512], BF16) for _ in range(3)]
    b_wsl = [Buf(f"wsl{i}") for i in range(3)]
    wg = A.t([128, 8, 8], BF16)
    b_wgs = Buf("wg")
    wo = A.t([128, 8, D], BF16)
    b_wo = Buf("wo")
    qT = A.t([128, 8, T], BF16)
    kT = A.t([128, 8, T], BF16)
    g1 = A.t([128, 8, T], BF16)
    m2 = A.t([128, 8, T], BF16)
    b_qT = [Buf(f"qT{c}") for c in range(8)]
    b_kT = [Buf(f"kT{c}") for c in range(8)]
    b_g1 = [Buf(f"g1{c}") for c in range(8)]
    b_m2 = [Buf(f"m2{c}") for c in range(8)]
    v = A.t([128, NSUB, D], BF16)
    b_v = [Buf(f"v{s}") for s in range(NSUB)]
    U = [A.t([128, T + 3], F32) for _ in range(2)]
    b_U = [Buf("U0"), Buf("U1")]
    acc = [A.t([128, T], F32) for _ in range(2)]
    b_acc = [Buf("acc0"), Buf("acc1")]
    ccs = [A.t([128, T], F32) for _ in range(2)]
    b_ccs = [Buf("ccs0"), Buf("ccs1")]
    sgt = [A.t([128, T], BF16) for _ in range(2)]
    b_sgt = [Buf("sgt0"), Buf("sgt1")]
    hnTok = A.t([128, NSUB, D], BF16)
    b_hn = [Buf(f"hn{s}") for s in range(NSUB)]
    mT = A.t([128, 8, T], BF16)
    b_mT = [Buf(f"mT{c}") for c in range(8)]
    ya = [A.t([128, T], BF16) for _ in range(2)]
    b_ya = [Buf("ya0"), Buf("ya1")]
    xf = A.t([128, XF], F32)
    Cst = xf[:, 0:2048].rearrange("p (j h d) -> p j h d", j=2, h=4)
    nS = xf[:, 2048:2056].rearrange("p (j h) -> p j h", j=2)
    haloU = xf[:, 2056:2104].rearrange("p (c t) -> p c t", c=16)
    haloU2 = xf[:, 2104:2120].rearrange("p (c t) -> p c t", c=8)
    b_C = [Buf(f"C{h}") for h in range(4)]
    b_nS = Buf("nS")
    b_hU = [Buf(f"hU{c}") for c in range(16)]
    b_hU2 = [Buf(f"hU2{c}") for c in range(8)]
    Cb = A.t([128, 2, 4, DH], BF16)
    b_Cb = [Buf(f"Cb{h}") for h in range(4)]
    nb = A.t([128, 2, 4], BF16)
    b_nb = Buf("nb")
    kS = [A.t([128, 4, DH], BF16) for _ in range(2)]
    b_kS = [Buf("kS0"), Buf("kS1")]
    Pm = [A.t([128, 4, 128], BF16) for _ in range(2)]
    b_Pm = [Buf("Pm0"), Buf("Pm1")]
    gates8 = A.t([128, NSUB, 8], F32)
    b_g8 = [Buf(f"g8{s}") for s in range(NSUB)]
    sm = [A.t([128, 96], F32) for _ in range(2)]
    b_sm = [Buf("sm0"), Buf("sm1")]
    rb = [A.t([128, D], F32) for _ in range(2)]
    b_rb = [Buf("rb0"), Buf("rb1")]
    G1 = A.t([128, D], F32)
    lng = A.t([128, D], F32)
    lnb = A.t([128, D], F32)
    bg = A.t([128, 8], F32)
    b_G1, b_lng, b_lnb, b_bg = Buf("G1"), Buf("lng"), Buf("lnb"), Buf("bg")
    rows = A.t([14, D], F32)
    b_rows = Buf("rows")
    cols = A.t([128, 8, 14], F32)
    b_cols = Buf("cols")
    sc1 = A.t([128, 8], F32)
    b_sc1 = Buf("sc1")
    cb = [k.b_const]

    k.dma("sp", rows[0:1, :], mod_l[:, 0:D], reads=[k.b_modd], writes=[b_rows])
    k.dma("sp", rows[1:2, :], mod_l[:, D:2 * D], reads=[k.b_modd], writes=[b_rows])
    k.dma("sp", rows[2:3, :], prm["hn_gain"], writes=[b_rows])
    k.dma("sp", rows[3:6, :], prm["conv_short"], writes=[b_rows])
    k.dma("sp", rows[6:10, :], prm["conv_qk"][:, 0:D], writes=[b_rows])
    k.dma("sp", rows[10:14, :], prm["conv_qk"][:, D:2 * D], writes=[b_rows])
    k.rows_to_cols(rows, 14, cols, b_rows, b_cols, PA[:, 0:512], bPA[0])
    k.dve(lambda e: e.tensor_scalar_add(sc1, cols[:, :, 1], 1.0), reads=[b_cols], writes=[b_sc1])
    k.dma("sp", G1, mod_l[:, 2 * D:3 * D].partition_broadcast(128), reads=[k.b_modd], writes=[b_G1])
    k.pool(lambda e: e.tensor_scalar_add(G1, G1, 1.0), reads=[b_G1], writes=[b_G1])
    k.dma("sp", lng, prm["ln_g"].partition_broadcast(128), writes=[b_lng])
    k.dma("sp", lnb, prm["ln_b"].partition_broadcast(128), writes=[b_lnb])
    k.dma("sp", bg, prm["b_gates"].partition_broadcast(128), writes=[b_bg])
    k.dma("sp", wo, W["wout"].rearrange("p (k n) -> p k n", k=8), reads=[W["b_wout"]], writes=[b_wo])
    k.dma("sp", wg, W["wg"].rearrange("p (k n) -> p k n", k=8), reads=[W["b_wg"]], writes=[b_wgs])
    b_xf_all = b_C + [b_nS] + b_hU + b_hU2
    if xfer_in is None:
        k.dve(lambda e: e.memset(xf, 0.0), reads=[], writes=b_xf_all)
    else:
        k.dma("sp", xf, xfer_in, reads=[k.b_xfer], writes=b_xf_all)
        k.dve(lambda e: e.tensor_scalar_mul(xf, xf, k.flag[:, 0:1]), reads=b_xf_all + [k.b_flag], writes=b_xf_all)
    for h in range(4):
        k.act(Cb[:, :, h, :], Cst[:, :, h, :], AF.Copy, reads=[b_C[h]], writes=[b_Cb[h]])
    k.act(nb, nS, AF.Copy, reads=[b_nS], writes=[b_nb])

    banks = [(PB[:, 0:512], bPB[0]), (PB[:, 512:1024], bPB[1]), (PC[:, 0:512], bPC[0]), (PC[:, 512:1024], bPC[1])]
    st = {"bank": 0, "w": 0, "u": 0}

    def next_bank():
        b = banks[st["bank"] % 4]
        st["bank"] += 1
        return b

    def load_w(blk, ncols=512):
        i = st["w"] % 3
        st["w"] += 1
        k.dma("sp", wsl[i][:, :, 0:ncols], W["win"][blk].rearrange("p (k n) -> p k n", k=8)[:, :, 0:ncols],
              reads=[W["b_win"][blk]], writes=[b_wsl[i]])
        return wsl[i], b_wsl[i]

    def proj_fm(wt, bw, c0):
        pbank, pb = next_bank()
        for kk in range(8):
            k.mm(pbank, wt[:, kk, c0:c0 + 128], hT[:, kk, :], kk == 0, kk == 7,
                 reads=[bw, b_hT[kk]], writes=[pb])
        return pbank, pb

    LN16 = float(np.log(16.0))
    pden = PD[:, 16:20]
    pbp = PD[:, 8:16]
    pdn = PD[:, 20:28].rearrange("p (j h) -> p j h", j=2)
    b_pg = [bPD[0]] * NSUB
    b_pbp = b_pden = b_pdn = bPD[0]
    PD1b = PD[:, 512:1024].bitcast(BF16)
    PA1b = PA[:, 512:1024].bitcast(BF16)
    b_PA1b = k.b_PA1

    for ti in range(k.NTILE):
        t0 = ti * T
        k.pump(k.pump_rate)
        k.dma("sp", xt, x_src[t0:t0 + T, :].rearrange("(s p) d -> p s d", p=128), writes=[b_xt])
        for c in range(8):
            hb = c % 2
            pbank = PA[:, hb * 512:(hb + 1) * 512]
            for s in range(NSUB):
                k.tr(pbank[:, s * 128:(s + 1) * 128], xt[:, s, c * 128:(c + 1) * 128], k.ident[:],
                     reads=[b_xt, k.b_const], writes=bPAh[hb])
            k.act(hT[:, c, :], pbank, AF.Identity, reads=bPAh[hb] + [b_sc1, b_cols], writes=[b_hT[c]],
                  scale=sc1[:, c:c + 1], bias=cols[:, c, 0:1])
        k.pool(lambda e: e.tensor_scalar_mul(xt, xt, ALPHA), reads=[b_xt], writes=[b_xt])
        for blk in range(4):
            wt, bw = load_w(blk)
            for cc in range(4):
                c16 = blk * 4 + cc
                c8 = c16 % 8
                rbase = 6 if c16 < 8 else 10
                dstT, b_dst = (qT, b_qT) if c16 < 8 else (kT, b_kT)
                pbank, pb = proj_fm(wt, bw, cc * 128)
                u = st["u"] % 2
                st["u"] += 1
                Ut, bU, ac, bac = U[u], b_U[u], acc[u], b_acc[u]
                k.pool(lambda e, Ut=Ut, c16=c16: e.tensor_copy(Ut[:, 0:3], haloU[:, c16, :]),
                       reads=[b_hU[c16]], writes=[bU])
                k.act(Ut[:, 3:T + 3], pbank, AF.Copy, reads=[pb], writes=[bU])
                k.pool(lambda e, Ut=Ut, c16=c16: e.tensor_copy(haloU[:, c16, :], Ut[:, T:T + 3]),
                       reads=[bU], writes=[b_hU[c16]])
                k.pool(lambda e, Ut=Ut, ac=ac, c8=c8, rbase=rbase: e.tensor_scalar_mul(
                    ac, Ut[:, 0:T], cols[:, c8, rbase:rbase + 1]), reads=[bU, b_cols], writes=[bac])
                for j in range(1, 4):
                    k.dve(lambda e, Ut=Ut, ac=ac, c8=c8, rbase=rbase, j=j: e.scalar_tensor_tensor(
                        ac, in0=Ut[:, j:j + T], scalar=cols[:, c8, rbase + j:rbase + j + 1], in1=ac,
                        op0=ALU.mult, op1=ALU.add), reads=[bU, b_cols, bac], writes=[bac])
                k.act(dstT[:, c8, :], ac, AF.Silu, reads=[bac], writes=[b_dst[c8]])
        for blk in (4, 5):
            wt, bw = load_w(blk)
            for s in range(NSUB):
                pbank, pb = next_bank()
                for kk in range(8):
                    k.mm(pbank, hT[:, kk, s * 128:(s + 1) * 128], wt[:, kk, :], kk == 0, kk == 7,
                         reads=[bw, b_hT[kk]], writes=[pb])
                k.act(v[:, s, (blk - 4) * 512:(blk - 3) * 512], pbank, AF.Copy, reads=[pb], writes=[b_v[s]])
        for blk in (6, 7):
            wt, bw = load_w(blk)
            for cc in range(4):
                c8 = (blk - 6) * 4 + cc
                pbank, pb = proj_fm(wt, bw, cc * 128)
                k.act(g1[:, c8, :], pbank, AF.Sigmoid, reads=[pb], writes=[b_g1[c8]])
        for s in range(NSUB):
            pg = PD[:, 32 + s * 8:32 + (s + 1) * 8]
            for kk in range(8):
                k.mm(pg, hT[:, kk, s * 128:(s + 1) * 128], wg[:, kk, :], kk == 0, kk == 7,
                     reads=[b_wgs, b_hT[kk]], writes=[b_pg[s]])
            k.dve(lambda e, s=s, pg=pg: e.tensor_tensor(gates8[:, s, :], pg, bg, op=ALU.add),
                  reads=[b_pg[s], b_bg], writes=[b_g8[s]])
        for j in range(8):
            wt, bw = load_w(8 + j, 384)
            pcb, pbcb = proj_fm(wt, bw, 0)
            pcc, pbcc = proj_fm(wt, bw, 128)
            pcx, pbcx = proj_fm(wt, bw, 256)
            u = st["u"] % 2
            st["u"] += 1
            Ut, bU, ac, bac, cs, bcs = U[u], b_U[u], acc[u], b_acc[u], ccs[u], b_ccs[u]
            k.act(cs, pcc, AF.Copy, reads=[pbcc], writes=[bcs])
            k.pool(lambda e, Ut=Ut, j=j: e.tensor_copy(Ut[:, 0:2], haloU2[:, j, :]), reads=[b_hU2[j]], writes=[bU])
            k.dve(lambda e, Ut=Ut, cs=cs, pcx=pcx: e.tensor_tensor(Ut[:, 2:T + 2], pcx, cs, op=ALU.mult),
                  reads=[pbcx, bcs], writes=[bU])
            k.pool(lambda e, Ut=Ut, j=j: e.tensor_copy(haloU2[:, j, :], Ut[:, T:T + 2]), reads=[bU], writes=[b_hU2[j]])
            k.pool(lambda e, Ut=Ut, ac=ac, j=j: e.tensor_scalar_mul(ac, Ut[:, 0:T], cols[:, j, 3:4]),
                   reads=[bU, b_cols], writes=[bac])
            for tp in (1, 2):
                k.dve(lambda e, Ut=Ut, ac=ac, j=j, tp=tp: e.scalar_tensor_tensor(
                    ac, in0=Ut[:, tp:tp + T], scalar=cols[:, j, 3 + tp:4 + tp], in1=ac, op0=ALU.mult, op1=ALU.add),
                    reads=[bU, b_cols, bac], writes=[bac])
            k.dve(lambda e, ac=ac, pcb=pcb, j=j: e.tensor_tensor(m2[:, j, :], ac, pcb, op=ALU.mult),
                  reads=[bac, pbcb], writes=[b_m2[j]])
        for blk in range(16, 20):
            wt, bw = load_w(blk)
            for cc in range(4):
                c8 = ((blk - 16) % 2) * 4 + cc
                tgt, b_tgt = (g1, b_g1) if blk < 18 else (m2, b_m2)
                pbank, pb = proj_fm(wt, bw, cc * 128)
                u = st["u"] % 2
                st["u"] += 1
                k.act(sgt[u], pbank, AF.Sigmoid, reads=[pb], writes=[b_sgt[u]])
                k.pool(lambda e, tgt=tgt, c8=c8, u=u: e.tensor_tensor(tgt[:, c8, :], tgt[:, c8, :], sgt[u], op=ALU.mult),
                       reads=[b_tgt[c8], b_sgt[u]], writes=[b_tgt[c8]])

        for s in range(NSUB):
            sub = slice(s * 128, (s + 1) * 128)
            z = (ti * NSUB + s) % 2
            S = sm[z]
            bS = b_sm[z]
            e1, l1, tmp4, ws_, ebt, ebL = S[:, 0:4], S[:, 4:8], S[:, 8:12], S[:, 12:16], S[:, 16:20], S[:, 20:24]
            d1, d2, scl, t1, t3, aa = S[:, 24:28], S[:, 28:32], S[:, 32:36], S[:, 36:40], S[:, 40:44], S[:, 44:48]
            stt = S[:, 48:72].rearrange("p (h x) -> p h x", h=4)
            mv = S[:, 72:80].rearrange("p (h x) -> p h x", h=4)
            k.act(e1, gates8[:, s, 4:8], AF.Exp, reads=[b_g8[s]], writes=[bS], scale=-1.0)
            k.act(l1, e1, AF.Ln, reads=[bS], writes=[bS], bias=1.0)
            k.mm(pbp[:, 0:4], k.triu[:], l1, True, True, reads=[bS, k.b_const], writes=[b_pbp])
            k.mm(pbp[:, 4:8], k.ones[:], l1, True, True, reads=[bS, k.b_const], writes=[b_pbp])
            k.dve(lambda e, tmp4=tmp4, s=s: e.tensor_tensor(tmp4, gates8[:, s, 0:4], pbp[:, 0:4], op=ALU.add),
                  reads=[b_g8[s], b_pbp], writes=[bS])
            k.act(ws_, tmp4, AF.Exp, reads=[bS], writes=[bS], bias=k.nl16[:, 0:1])
            k.act(ebt, pbp[:, 0:4], AF.Exp, reads=[b_pbp], writes=[bS], scale=-1.0)
            k.act(ebL, pbp[:, 4:8], AF.Exp, reads=[b_pbp], writes=[bS], scale=-1.0)
            for h in range(4):
                for j in range(2):
                    k.tr(PD1b[:, (h * 2 + j) * 128:(h * 2 + j + 1) * 128], kT[:, 2 * h + j, sub], k.identb[:],
                         reads=[b_kT[2 * h + j], k.b_const], writes=[bPD[1]])
            for h in range(4):
                k.dve(lambda e, h=h, z=z, ws_=ws_: e.tensor_scalar_mul(kS[z][:, h, :], PD1b[:, h * 256:(h + 1) * 256],
                                                                       ws_[:, h:h + 1]),
                      reads=[bPD[1], bS], writes=[b_kS[z]])
            for h in range(4):
                for j in range(2):
                    k.mm(PA[:, h * 128:(h + 1) * 128], kT[:, 2 * h + j, sub], qT[:, 2 * h + j, sub], j == 0, j == 1,
                         reads=[b_kT[2 * h + j], b_qT[2 * h + j]], writes=[bPA[0]])
            for h in range(4):
                k.dve(lambda e, h=h, z=z, ws_=ws_: e.scalar_tensor_tensor(
                    Pm[z][:, h, :], in0=PA[:, h * 128:(h + 1) * 128], scalar=ws_[:, h:h + 1], in1=k.triu[:],
                    op0=ALU.mult, op1=ALU.mult), reads=[bPA[0], bS, k.b_const], writes=[b_Pm[z]])
            for h in range(4):
                hb = h // 2
                nump = PB[:, h * 256:(h + 1) * 256]
                k.mm(nump, Pm[z][:, h, :], v[:, s, h * 256:(h + 1) * 256], True, False,
                     reads=[b_Pm[z], b_v[s]], writes=[bPB[hb]])
                for j in range(2):
                    k.mm(nump, qT[:, 2 * h + j, sub], Cb[:, j, h, :], False, j == 1,
                         reads=[b_qT[2 * h + j], b_Cb[h]], writes=[bPB[hb]])
                k.mm(pden[:, h:h + 1], Pm[z][:, h, :], k.onesb[:, 0:1], True, False,
                     reads=[b_Pm[z], k.b_const], writes=[b_pden])
                for j in range(2):
                    k.mm(pden[:, h:h + 1], qT[:, 2 * h + j, sub], nb[:, j, h:h + 1], False, j == 1,
                         reads=[b_qT[2 * h + j], b_nb], writes=[b_pden])
            for h in range(4):
                hb = h % 2
                pdel = PC[:, hb * 512:(hb + 1) * 512]
                for j in range(2):
                    k.mm(pdel[:, j * 256:(j + 1) * 256], kS[z][:, h, j * 128:(j + 1) * 128],
                         v[:, s, h * 256:(h + 1) * 256], True, True, reads=[b_kS[z], b_v[s]], writes=[bPC[hb]])
                    k.mm(pdn[:, j, h:h + 1], kS[z][:, h, j * 128:(j + 1) * 128], k.onesb[:, 0:1], True, True,
                         reads=[b_kS[z], k.b_const], writes=[b_pdn])
                k.pool(lambda e, h=h, ebL=ebL: e.tensor_scalar_mul(Cst[:, :, h, :], Cst[:, :, h, :], ebL[:, h:h + 1]),
                       reads=[b_C[h], bS], writes=[b_C[h]])
                k.dve(lambda e, h=h, ebL=ebL, pdel=pdel: e.scalar_tensor_tensor(
                    Cst[:, :, h, :], in0=pdel.rearrange("p (j d) -> p j d", j=2), scalar=ebL[:, h:h + 1],
                    in1=Cst[:, :, h, :], op0=ALU.mult, op1=ALU.add), reads=[bPC[hb], bS, b_C[h]], writes=[b_C[h]])
                k.act(Cb[:, :, h, :], Cst[:, :, h, :], AF.Copy, reads=[b_C[h]], writes=[b_Cb[h]])
            k.dve(lambda e: e.tensor_tensor(nS, nS, pdn, op=ALU.add), reads=[b_nS, b_pdn], writes=[b_nS])
            k.dve(lambda e, ebL=ebL: e.tensor_tensor(nS, nS, ebL.unsqueeze(1).to_broadcast([128, 2, 4]), op=ALU.mult),
                  reads=[b_nS, bS], writes=[b_nS])
            k.act(nb, nS, AF.Copy, reads=[b_nS], writes=[b_nb])
            k.dve(lambda e, d1=d1, ebt=ebt: e.tensor_tensor(d1, pden, ebt, op=ALU.mult), reads=[b_pden, bS], writes=[bS])
            k.act(d2, d1, AF.Abs, reads=[bS], writes=[bS])
            k.dve(lambda e, d2=d2: e.tensor_scalar_max(d2, d2, 1.0), reads=[bS], writes=[bS])
            k.dve(lambda e, d2=d2: e.reciprocal(d2, d2), reads=[bS], writes=[bS])
            k.dve(lambda e, d2=d2, scl=scl, ebt=ebt: e.tensor_tensor(scl, ebt, d2, op=ALU.mult), reads=[bS], writes=[bS])
            for h in range(4):
                k.dve(lambda e, h=h, stt=stt: e.bn_stats(stt[:, h, :], PB[:, h * 256:(h + 1) * 256]),
                      reads=[bPB[h // 2]], writes=[bS])
                k.dve(lambda e, h=h, stt=stt, mv=mv: e.bn_aggr(mv[:, h, :], stt[:, h, :]), reads=[bS], writes=[bS])
            k.dve(lambda e, t1=t1, scl=scl: e.tensor_tensor(t1, scl, scl, op=ALU.mult), reads=[bS], writes=[bS])
            k.dve(lambda e, t1=t1, mv=mv: e.tensor_tensor(t1, t1, mv[:, :, 1], op=ALU.mult), reads=[bS], writes=[bS])
            k.act(t3, t1, AF.Sqrt, reads=[bS, k.b_const], writes=[bS], bias=k.epsc[:, 0:1])
            k.dve(lambda e, t3=t3: e.reciprocal(t3, t3), reads=[bS], writes=[bS])
            k.dve(lambda e, t3=t3, aa=aa, scl=scl: e.tensor_tensor(aa, t3, scl, op=ALU.mult), reads=[bS], writes=[bS])
            for h in range(4):
                k.dve(lambda e, h=h, s=s, mv=mv, aa=aa: e.tensor_scalar(
                    hnTok[:, s, h * 256:(h + 1) * 256], PB[:, h * 256:(h + 1) * 256], mv[:, h, 0:1], aa[:, h:h + 1],
                    op0=ALU.subtract, op1=ALU.mult), reads=[bPB[h // 2], bS], writes=[b_hn[s]])

        for c in range(8):
            hb = c % 2
            for s in range(NSUB):
                k.tr(PA1b[:, hb * 512 + s * 128:hb * 512 + (s + 1) * 128], hnTok[:, s, c * 128:(c + 1) * 128], k.identb[:],
                     reads=[b_hn[s], k.b_const], writes=[b_PA1b[hb]])
            k.dve(lambda e, c=c, hb=hb: e.scalar_tensor_tensor(
                ya[hb], in0=PA1b[:, hb * 512:(hb + 1) * 512], scalar=cols[:, c, 2:3], in1=g1[:, c, :],
                op0=ALU.mult, op1=ALU.mult), reads=[b_PA1b[hb], b_cols, b_g1[c]], writes=[b_ya[hb]])
            k.pool(lambda e, c=c, hb=hb: e.tensor_tensor(mT[:, c, :], ya[hb], m2[:, c, :], op=ALU.add),
                   reads=[b_ya[hb], b_m2[c]], writes=[b_mT[c]])

        for s in range(NSUB):
            sub = slice(s * 128, (s + 1) * 128)
            yb_, byb = (PB, bPB) if s % 2 == 0 else (PC, bPC)
            z = s % 2
            for n in range(2):
                for c in range(8):
                    k.mm(yb_[:, n * 512:(n + 1) * 512], mT[:, c, sub], wo[:, c, n * 512:(n + 1) * 512], c == 0, c == 7,
                         reads=[b_mT[c], b_wo], writes=[byb[n]])
            ln_epilogue(k, yb_[:], byb, rb[z], b_rb[z], xt[:, s, :], b_xt, G1, b_G1, lng, b_lng, lnb, b_lnb,
                        sm[z][:, 80:96], b_sm[z], x_dst[t0 + s * 128:t0 + (s + 1) * 128, :])
    if xfer_out is not None:
        o = k.dma("sp", xfer_out, xf, reads=b_xf_all, writes=[k.b_xfer])
        k.outs.append(o)
    P.barrier()


def ln_epilogue(k, yps, byps, r, b_r, xres, b_xres, G, b_G, lng, b_lng, lnb, b_lnb, S, bS, dst):
    st6 = S[:, 0:12].rearrange("p (a b) -> p a b", a=2)
    mv = S[:, 12:14]
    sq = S[:, 14:15]
    nmr = S[:, 15:16]
    if yps is not None:
        k.dve(lambda e: e.tensor_tensor(r, yps, G, op=ALU.mult), reads=byps + [b_G], writes=[b_r])
    k.pool(lambda e: e.tensor_tensor(r, r, xres, op=ALU.add), reads=[b_r, b_xres], writes=[b_r])
    for a in range(2):
        k.dve(lambda e, a=a: e.bn_stats(st6[:, a, :], r[:, a * 512:(a + 1) * 512]), reads=[b_r], writes=[bS])
    k.dve(lambda e: e.bn_aggr(mv, S[:, 0:12]), reads=[bS], writes=[bS])
    k.act(sq, mv[:, 1:2], AF.Sqrt, reads=[bS, k.b_const], writes=[bS], bias=k.epsc[:, 0:1])
    k.dve(lambda e: e.reciprocal(sq, sq), reads=[bS], writes=[bS])
    k.dve(lambda e: e.tensor_scalar(nmr, mv[:, 0:1], -1.0, sq, op0=ALU.mult, op1=ALU.mult), reads=[bS], writes=[bS])
    k.act(r, r, AF.Identity, reads=[b_r, bS], writes=[b_r], scale=sq, bias=nmr)
    k.pool(lambda e: e.tensor_tensor(r, r, lng, op=ALU.mult), reads=[b_r, b_lng], writes=[b_r])
    k.pool(lambda e: e.tensor_tensor(r, r, lnb, op=ALU.add), reads=[b_r, b_lnb], writes=[b_r])
    o = k.dma("sp", dst, r, reads=[b_r])
    k.outs.append(o)
    return o


def conv_ffn_weights(k, tag, w13_l, w2_l):
    W = {}
    W["w13"] = k.dint(f"w13b_{tag}", [14, 128, 8 * 2 * 256], BF16)
    W["w2"] = k.dint(f"w2b_{tag}", [2, 128, NF * 512], BF16)
    W["b_w13"] = [Buf(f"w13b{g}") for g in range(14)]
    W["b_w2"] = [Buf("w2b0"), Buf("w2b1")]
    for g in range(14):
        dv = W["w13"][g].rearrange("p (k i n) -> p k i n", k=8, i=2)
        for i in range(2):
            k.defer("gq", dv[:, :, i, :], w13_l[:, i * DFF + g * 256:i * DFF + (g + 1) * 256].rearrange(
                "(k p) n -> p k n", p=128), writes=[W["b_w13"][g]])
    for n in range(2):
        dv = W["w2"][n].rearrange("p (f c) -> p f c", f=NF)
        for q in range(4):
            k.defer("gq", dv[:, q * 7:(q + 1) * 7, :],
                  w2_l[q * 7 * 128:(q + 1) * 7 * 128, n * 512:(n + 1) * 512].rearrange("(f p) c -> p f c", p=128),
                  writes=[W["b_w2"][n]])
    return W


def phase_ffn(k, x_src, x_dst, mod_l, prm, experts, router_d, final=False):
    A = k.A
    A.reset()
    P = k.P
    PA, PB, PC, PD = k.psum
    bPA, bPB, bPC, bPD = k.pbuf
    bPAh = [[bPA[0]], [bPA[1]]]
    moe = router_d is not None
    xt = A.t([128, NSUB, D], F32)
    b_xt = Buf("xt")
    hT = A.t([128, 8, T], BF16)
    b_hT = [Buf(f"hT{c}") for c in range(8)]
    g = A.t([128, NF, T], BF16)
    b_g = [Buf(f"g{f}") for f in range(NF)]
    yacc = A.t([128, NSUB, D], F32)
    b_y = [Buf(f"y{s}") for s in range(NSUB)]
    w13s = [A.t([128, 8, 2, 256], BF16) for _ in range(2)]
    b_w13s = [Buf("w13s0"), Buf("w13s1")]
    w2s = [A.t([128, NF, 512], BF16) for _ in range(2)]
    b_w2s = [Buf("w2s0"), Buf("w2s1")]
    G2 = A.t([128, D], F32)
    lng = A.t([128, D], F32)
    lnb = A.t([128, D], F32)
    b_G2, b_lng, b_lnb = Buf("G2"), Buf("lng"), Buf("lnb")
    sa = [A.t([128, T], F32) for _ in range(2)]
    b_sa = [Buf("sa0"), Buf("sa1")]
    rows = A.t([2, D], F32)
    b_rows = Buf("rows")
    cols = A.t([128, 8, 2], F32)
    b_cols = Buf("cols")
    sc1 = A.t([128, 8], F32)
    b_sc1 = Buf("sc1")
    sm = [A.t([128, 16], F32) for _ in range(2)]
    b_sm = [Buf("sm0"), Buf("sm1")]
    if moe:
        hT32 = A.t([128, 8, T], F32)
        b_hT32 = [Buf(f"hT32{c}") for c in range(8)]
        wr = A.t([128, 8, NE], F32)
        b_wr = Buf("wr")
        gates = A.t([128, NSUB, NE], F32)
        b_gates = [Buf(f"gates{s}") for s in range(NSUB)]
        rt = [A.t([128, 64], F32) for _ in range(2)]
        b_rt = [Buf("rt0"), Buf("rt1")]

    k.dma("sp", rows[0:1, :], mod_l[:, 3 * D:4 * D], reads=[k.b_modd], writes=[b_rows])
    k.dma("sp", rows[1:2, :], mod_l[:, 4 * D:5 * D], reads=[k.b_modd], writes=[b_rows])
    k.rows_to_cols(rows, 2, cols, b_rows, b_cols, PA[:, 0:512], bPA[0])
    k.dve(lambda e: e.tensor_scalar_add(sc1, cols[:, :, 1], 1.0), reads=[b_cols], writes=[b_sc1])
    k.dma("sp", G2, mod_l[:, 5 * D:6 * D].partition_broadcast(128), reads=[k.b_modd], writes=[b_G2])
    k.pool(lambda e: e.tensor_scalar_add(G2, G2, 1.0), reads=[b_G2], writes=[b_G2])
    k.dma("sp", lng, prm["ln_g"].partition_broadcast(128), writes=[b_lng])
    k.dma("sp", lnb, prm["ln_b"].partition_broadcast(128), writes=[b_lnb])
    if moe:
        k.dma("sp", wr, router_d.rearrange("(k p) e -> p k e", p=128), writes=[b_wr])

    banks = [(PB[:, 0:512], bPB[0]), (PB[:, 512:1024], bPB[1]), (PC[:, 0:512], bPC[0]), (PC[:, 512:1024], bPC[1])]
    st = {"bank": 0, "w13": 0, "w2": 0, "u": 0}

    def next_bank():
        b = banks[st["bank"] % 4]
        st["bank"] += 1
        return b

    for ti in range(k.NTILE):
        t0 = ti * T
        k.pump(k.pump_rate)
        k.dma("sp", xt, x_src[t0:t0 + T, :].rearrange("(s p) d -> p s d", p=128), writes=[b_xt])
        for c in range(8):
            hb = c % 2
            pbank = PA[:, hb * 512:(hb + 1) * 512]
            for s in range(NSUB):
                k.tr(pbank[:, s * 128:(s + 1) * 128], xt[:, s, c * 128:(c + 1) * 128], k.ident[:],
                     reads=[b_xt, k.b_const], writes=bPAh[hb])
            k.act(hT[:, c, :], pbank, AF.Identity, reads=bPAh[hb] + [b_sc1, b_cols], writes=[b_hT[c]],
                  scale=sc1[:, c:c + 1], bias=cols[:, c, 0:1])
            if moe and "H" not in _DBG:
                k.act(hT32[:, c, :], pbank, AF.Identity, reads=bPAh[hb] + [b_sc1, b_cols], writes=[b_hT32[c]],
                      scale=sc1[:, c:c + 1], bias=cols[:, c, 0:1])
        k.pool(lambda e: e.tensor_scalar_mul(xt, xt, ALPHA), reads=[b_xt], writes=[b_xt])
        if moe and "R" in _DBG:
            for s in range(NSUB):
                k.dve(lambda e, s=s: e.memset(gates[:, s, :], 0.125), reads=[], writes=[b_gates[s]])
        elif moe:
            for s in range(NSUB):
                z = s % 2
                R = rt[z]
                bR = b_rt[z]
                lg, mx8, m1k, m2k, dd, p1, p2 = (R[:, 0:8], R[:, 8:16], R[:, 16:24], R[:, 24:32], R[:, 32:33],
                                                 R[:, 33:34], R[:, 34:35])
                pl = PD[:, s * 8:(s + 1) * 8]
                if "M" in _DBG:
                    k.dve(lambda e, lg=lg, s=s: e.tensor_copy(lg, hT32[:, 0, s * 8:(s + 1) * 8]), reads=[b_hT32[0]], writes=[bR])
                else:
                    for kk in range(8):
                        k.mm(pl, hT32[:, kk, s * 128:(s + 1) * 128], wr[:, kk, :], kk == 0, kk == 7,
                             reads=[b_hT32[kk], b_wr], writes=[bPD[0]])
                    k.dve(lambda e, lg=lg, pl=pl: e.tensor_copy(lg, pl), reads=[bPD[0]], writes=[bR])
                k.dve(lambda e, lg=lg, mx8=mx8: e.max(mx8, lg), reads=[bR], writes=[bR])
                k.dve(lambda e, lg=lg, mx8=mx8, m1k=m1k: e.tensor_scalar(m1k, lg, mx8[:, 0:1], None, op0=ALU.is_equal),
                      reads=[bR], writes=[bR])
                k.dve(lambda e, lg=lg, mx8=mx8, m2k=m2k: e.tensor_scalar(m2k, lg, mx8[:, 1:2], None, op0=ALU.is_equal),
                      reads=[bR], writes=[bR])
                k.dve(lambda e, mx8=mx8, dd=dd: e.tensor_tensor(dd, mx8[:, 1:2], mx8[:, 0:1], op=ALU.subtract),
                      reads=[bR], writes=[bR])
                k.act(p2, dd, AF.Exp, reads=[bR], writes=[bR])
                k.dve(lambda e, p1=p1, p2=p2: e.tensor_scalar_add(p1, p2, 1.0), reads=[bR], writes=[bR])
                k.dve(lambda e, p1=p1: e.reciprocal(p1, p1), reads=[bR], writes=[bR])
                k.dve(lambda e, p1=p1, p2=p2: e.tensor_tensor(p2, p2, p1, op=ALU.mult), reads=[bR], writes=[bR])
                k.dve(lambda e, s=s, m1k=m1k, p1=p1: e.tensor_scalar_mul(gates[:, s, :], m1k, p1),
                      reads=[bR], writes=[b_gates[s]])
                k.dve(lambda e, s=s, m2k=m2k, p2=p2: e.scalar_tensor_tensor(
                    gates[:, s, :], in0=m2k, scalar=p2, in1=gates[:, s, :], op0=ALU.mult, op1=ALU.add),
                    reads=[bR, b_gates[s]], writes=[b_gates[s]])
        for ei, We in enumerate(experts):
            for gi in range(14):
                i = st["w13"] % 2
                st["w13"] += 1
                k.dma("sp", w13s[i], We["w13"][gi].rearrange("p (k i n) -> p k i n", k=8, i=2),
                      reads=[We["b_w13"][gi]], writes=[b_w13s[i]])
                for i2 in range(2):
                    f = gi * 2 + i2
                    pa, pba = next_bank()
                    for kk in range(8):
                        k.mm(pa, w13s[i][:, kk, 0, i2 * 128:(i2 + 1) * 128], hT[:, kk, :], kk == 0, kk == 7,
                             reads=[b_w13s[i], b_hT[kk]], writes=[pba])
                    pb_, pbb = next_bank()
                    for kk in range(8):
                        k.mm(pb_, w13s[i][:, kk, 1, i2 * 128:(i2 + 1) * 128], hT[:, kk, :], kk == 0, kk == 7,
                             reads=[b_w13s[i], b_hT[kk]], writes=[pbb])
                    u = st["u"] % 2
                    st["u"] += 1
                    k.act(sa[u], pa, AF.Silu, reads=[pba], writes=[b_sa[u]])
                    k.dve(lambda e, f=f, u=u, pb_=pb_: e.tensor_tensor(g[:, f, :], sa[u], pb_, op=ALU.mult),
                          reads=[b_sa[u], pbb], writes=[b_g[f]])
            for n in range(2):
                i = st["w2"] % 2
                st["w2"] += 1
                k.dma("sp", w2s[i], We["w2"][n].rearrange("p (f c) -> p f c", f=NF),
                      reads=[We["b_w2"][n]], writes=[b_w2s[i]])
                for s in range(NSUB):
                    py, pby = next_bank()
                    for f in range(NF):
                        k.mm(py, g[:, f, s * 128:(s + 1) * 128], w2s[i][:, f, :], f == 0, f == NF - 1,
                             reads=[b_g[f], b_w2s[i]], writes=[pby])
                    ydst = yacc[:, s, n * 512:(n + 1) * 512]
                    if not moe:
                        k.dve(lambda e, ydst=ydst, py=py, n=n: e.tensor_tensor(ydst, py, G2[:, n * 512:(n + 1) * 512],
                                                                               op=ALU.mult),
                              reads=[pby, b_G2], writes=[b_y[s]])
                    elif ei == 0:
                        k.dve(lambda e, ydst=ydst, py=py, s=s, ei=ei: e.tensor_scalar_mul(ydst, py, gates[:, s, ei:ei + 1]),
                              reads=[pby, b_gates[s]], writes=[b_y[s]])
                    else:
                        k.dve(lambda e, ydst=ydst, py=py, s=s, ei=ei: e.scalar_tensor_tensor(
                            ydst, in0=py, scalar=gates[:, s, ei:ei + 1], in1=ydst, op0=ALU.mult, op1=ALU.add),
                            reads=[pby, b_gates[s], b_y[s]], writes=[b_y[s]])
        for s in range(NSUB):
            z = s % 2
            if moe:
                k.pool(lambda e, s=s: e.tensor_tensor(yacc[:, s, :], yacc[:, s, :], G2, op=ALU.mult),
                       reads=[b_y[s], b_G2], writes=[b_y[s]])
            o = ln_epilogue(k, None, None, yacc[:, s, :], b_y[s], xt[:, s, :], b_xt, None, None, lng, b_lng, lnb, b_lnb,
                            sm[z], b_sm[z], x_dst[t0 + s * 128:t0 + (s + 1) * 128, :])
    P.barrier()


NCORES = 8
NT_CORE = 4096
DEPTH = 4
FUSED = False
_PROGS = {}


def _prm_inputs(k, sub):
    p = {"ln_g": k.din("ln_g", [1, D]), "ln_b": k.din("ln_b", [1, D])}
    if sub == 0:
        p.update({"b_gates": k.din("b_gates", [1, 8]), "conv_qk": k.din("conv_qk", [4, 2 * D]),
                  "hn_gain": k.din("hn_gain", [1, D]), "conv_short": k.din("conv_short", [3, D])})
    return p


def build_p0():
    k = K(NT_CORE)
    c_d = k.din("c", [1, D])
    w_ada = k.din("w_ada", [DEPTH, D, 6 * D])
    b_ada = k.din("b_ada", [DEPTH, 6 * D])
    modd = k.dout("modd", [DEPTH, 6 * D])
    k.P.barrier()
    phase_p0(k, c_d, w_ada, b_ada, modd, DEPTH)
    return k.finish()


def build_mix():
    k = K(NT_CORE)
    x_d = k.din("x", [NT_CORE, D])
    modd = k.din("modd", [1, 6 * D])
    w_in = k.din("w_in", [D, PW])
    w_out = k.din("w_out", [D, D])
    prm = _prm_inputs(k, 0)
    xfer_in = k.din("xfer_in", [128, XF])
    flag_d = k.din("flag", [128, 1])
    x_o = k.dout("x_out", [NT_CORE, D])
    xfer_out = k.dout("xfer_out", [128, XF])
    k.load_flag(flag_d)
    k.P.barrier()
    W = conv_mixer_weights(k, "m", w_in, w_out)
    k.flush()
    phase_mixer(k, x_d, x_o, modd, prm, W, xfer_in, xfer_out)
    return k.finish()


def build_ffn(moe):
    k = K(NT_CORE)
    x_d = k.din("x", [NT_CORE, D])
    modd = k.din("modd", [1, 6 * D])
    prm = _prm_inputs(k, 1)
    x_o = k.dout("x_out", [NT_CORE, D])
    k.P.barrier()
    if moe:
        w13 = k.din("w13", [NE, D, 2 * DFF])
        w2 = k.din("w2", [NE, DFF, D])
        wr = k.din("wr", [D, NE])
        Ws = [conv_ffn_weights(k, f"e{e}", w13[e], w2[e]) for e in range(NE)]
        k.flush()
        phase_ffn(k, x_d, x_o, modd, prm, Ws, wr)
    else:
        w13 = k.din("w13", [D, 2 * DFF])
        w2 = k.din("w2", [DFF, D])
        Ws = [conv_ffn_weights(k, "d", w13, w2)]
        k.flush()
        phase_ffn(k, x_d, x_o, modd, prm, Ws, None)
    return k.finish()


def _prog(name):
    if name not in _PROGS:
        _PROGS[name] = {"p0": build_p0, "mix": build_mix, "ffn_d": lambda: build_ffn(False),
                        "ffn_m": lambda: build_ffn(True)}[name]()
    return _PROGS[name]


def _run(nc, in_maps):
    res = run_bass_kernel_spmd(nc, in_maps, core_ids=list(range(NCORES)))
    return res.results


def kernel_unfused(inp):
    f32 = lambda a: np.ascontiguousarray(np.asarray(a, dtype=np.float32))
    x = f32(inp["x"])
    B, S, _ = x.shape
    half = S // 2
    xs = [x[c // 2, (c % 2) * half:(c % 2 + 1) * half] for c in range(NCORES)]
    cc = f32(inp["c"])
    w_ada, b_ada = f32(inp["w_ada"]), f32(inp["b_ada"])
    r = _run(_prog("p0"), [{"c": cc[c // 2:c // 2 + 1], "w_ada": w_ada, "b_ada": b_ada} for c in range(NCORES)])
    modd = [r[c]["modd"] for c in range(NCORES)]
    zer = np.zeros((128, XF), np.float32)
    for l in range(DEPTH):
        base = {"w_in": f32(inp["w_in"][l]), "w_out": f32(inp["w_out"][l]), "b_gates": f32(inp["b_gates"][l])[None],
                "conv_qk": f32(inp["conv_qk"][l]), "hn_gain": f32(inp["hn_gain"][l])[None],
                "conv_short": f32(inp["conv_short"][l]), "ln_g": f32(inp["ln_g"][l, 0])[None],
                "ln_b": f32(inp["ln_b"][l, 0])[None]}
        xfer = [zer] * NCORES
        for p in range(2):
            maps = [dict(base, x=xs[c], modd=modd[c][l:l + 1], xfer_in=xfer[c],
                         flag=np.full((128, 1), float(c % 2) if p == 1 else 0.0, np.float32)) for c in range(NCORES)]
            r = _run(_prog("mix"), maps)
            xfer = [r[c & ~1]["xfer_out"] for c in range(NCORES)]
        xm = [r[c]["x_out"] for c in range(NCORES)]
        fb = {"ln_g": f32(inp["ln_g"][l, 1])[None], "ln_b": f32(inp["ln_b"][l, 1])[None]}
        if l % 2 == 0:
            fb.update(w13=f32(inp["dense_w13"][l // 2]), w2=f32(inp["dense_w2"][l // 2]))
            name = "ffn_d"
        else:
            fb.update(w13=f32(inp["moe_w13"][l // 2]), w2=f32(inp["moe_w2"][l // 2]), wr=f32(inp["w_router"][l // 2]))
            name = "ffn_m"
        r = _run(_prog(name), [dict(fb, x=xm[c], modd=modd[c][l:l + 1]) for c in range(NCORES)])
        xs = [r[c]["x_out"] for c in range(NCORES)]
    out = np.empty((B, S, D), np.float32)
    for c in range(NCORES):
        out[c // 2, (c % 2) * half:(c % 2 + 1) * half] = xs[c]
    return out


def kernel(**inputs):
    return kernel_unfused(inputs)
```

```python
import contextlib
import numpy as np
import concourse.bass as bass
import concourse.mybir as mybir
from concourse.bass_utils import run_bass_kernel_spmd

F32 = mybir.dt.float32
BF16 = mybir.dt.bfloat16
U8 = mybir.dt.uint8
AF = mybir.ActivationFunctionType
ALU = mybir.AluOpType

D = 1024
H = 4
DH = 256
PW = 9224
DFF = 3584
NE = 8
NF = DFF // 128
LN_EPS = 1e-5
T = 512
NSUB = 4

import os
_DBG = os.environ.get("FFN_DBG", "")
COMPUTE = ("pe", "act", "dve", "pool")
QUEUES = ("sp", "gq", "cc")
QINC = {"sp": 16, "gq": 16, "cc": 1}


class Buf:
    __slots__ = ("name", "writer", "readers")

    def __init__(self, name):
        self.name = name
        self.writer = None
        self.readers = []


class Op:
    __slots__ = ("eng", "fn", "deps", "is_dma", "signal", "count", "sem", "stream")

    def __init__(self, eng, fn, is_dma, stream):
        self.eng = eng
        self.fn = fn
        self.deps = []
        self.is_dma = is_dma
        self.signal = False
        self.count = 0
        self.sem = None
        self.stream = stream


class Prog:
    def __init__(self, nc, n_dma_sems=12):
        self.nc = nc
        self.ops = {e: [] for e in COMPUTE + ("sp",)}
        self.n_dma_sems = n_dma_sems
        self.all_ops = []
        self.bar = []
        self.need_bar = set()
        self.dmas_since_bar = []

    def op(self, eng, fn, reads=(), writes=()):
        is_dma = eng in QUEUES
        stream = "pool" if eng in ("gq", "cc") else eng
        o = Op(eng, fn, is_dma, stream)
        deps = []
        pe = stream == "pe"
        for b in reads:
            w = b.writer
            if w is not None and (w.is_dma or w.stream != stream or not pe):
                deps.append(w)
        for b in writes:
            w = b.writer
            if w is not None and (w.is_dma or w.stream != stream or not pe):
                deps.append(w)
            for r in b.readers:
                if r.is_dma or r.stream != stream or not pe:
                    deps.append(r)
        if stream in self.need_bar:
            deps.extend(self.bar)
            self.need_bar.discard(stream)
        o.deps = deps
        for b in reads:
            b.readers.append(o)
        for b in writes:
            b.writer = o
            b.readers = []
        self.all_ops.append(o)
        self.ops[stream].append(o)
        if is_dma:
            self.dmas_since_bar.append(o)
        return o

    def barrier(self):
        bar = list(self.dmas_since_bar)
        for s, lst in self.ops.items():
            for o in reversed(lst):
                if not o.is_dma:
                    bar.append(o)
                    break
        self.bar = bar
        self.need_bar = set(self.ops.keys())
        self.dmas_since_bar = []

    def emit(self, final_wait_ops=()):
        nc = self.nc
        streams = list(self.ops.keys())
        for o in self.all_ops:
            for d in o.deps:
                d.signal = True
        for o in final_wait_ops:
            o.signal = True
        with contextlib.ExitStack() as es:
            esem = {s: es.enter_context(nc.semaphore("s_" + s)) for s in streams}
            dsem = {q: [es.enter_context(nc.semaphore(f"d_{q}{i}")) for i in range(self.n_dma_sems)]
                    for q in QUEUES}
            ecount = {s: 0 for s in streams}
            dcount = {q: [0] * self.n_dma_sems for q in QUEUES}
            drr = {q: 0 for q in QUEUES}
            dprev = {q: [None] * self.n_dma_sems for q in QUEUES}
            for o in self.all_ops:
                if o.is_dma:
                    q = o.eng
                    i = drr[q]
                    drr[q] = (i + 1) % self.n_dma_sems
                    prev = dprev[q][i]
                    if prev is not None:
                        o.deps.append(prev)
                    dcount[q][i] += QINC[q]
                    o.sem = dsem[q][i]
                    o.count = dcount[q][i]
                    dprev[q][i] = o
                    o.signal = True
                elif o.signal:
                    ecount[o.stream] += 1
                    o.sem = esem[o.stream]
                    o.count = ecount[o.stream]
            self.n_waits = 0
            blk = es.enter_context(nc.Block())

            def run_stream(stream):
                def body(e):
                    waited = {}
                    for o in self.ops[stream]:
                        need = {}
                        for d in o.deps:
                            k = d.sem.name
                            if need.get(k, (None, 0))[1] < d.count:
                                need[k] = (d.sem, d.count)
                        for k, (sem, cnt) in need.items():
                            if waited.get(k, 0) < cnt:
                                e.wait_ge(sem, cnt)
                                waited[k] = cnt
                                self.n_waits += 1
                        ins = o.fn(e)
                        if o.signal:
                            ins.then_inc(o.sem, QINC[o.eng] if o.is_dma else 1)
                    if stream == "sp":
                        for o in final_wait_ops:
                            if waited.get(o.sem.name, 0) < o.count:
                                e.wait_ge(o.sem, o.count)
                                waited[o.sem.name] = o.count
                return body

            blk.tensor(run_stream("pe"))
            blk.scalar(run_stream("act"))
            blk.vector(run_stream("dve"))
            blk.gpsimd(run_stream("pool"))
            blk.sync(run_stream("sp"))


def _dtsize(dt):
    return {F32: 4, BF16: 2, U8: 1}[dt]


class Arena:
    def __init__(self, ap_u8, nbytes):
        self.base = ap_u8
        self.nbytes = nbytes
        self.off = 0

    def reset(self):
        self.off = 0

    def t(self, shape, dt):
        n = 1
        for s in shape[1:]:
            n *= s
        nb = n * _dtsize(dt)
        off = (self.off + 63) // 64 * 64
        assert off + nb <= self.nbytes, f"arena overflow: need {off + nb} > {self.nbytes}"
        self.off = off + nb
        v = self.base[:, off:off + nb].bitcast(dt)
        if len(shape) == 3:
            v = v.rearrange("p (a b) -> p a b", a=shape[1])
        elif len(shape) == 4:
            v = v.rearrange("p (a b c) -> p a b c", a=shape[1], b=shape[2])
        if shape[0] != 128:
            v = v[0:shape[0]]
        return v


class K:
    def __init__(self, NT):
        self.NT = NT
        self.NTILE = NT // T
        self.nc = bass.Bass("TRN2", target_bir_lowering=False)
        self.P = Prog(self.nc)
        self.es = contextlib.ExitStack()
        self.outs = []
        self.deferred = []
        self.pump_rate = 0
        nc = self.nc
        es = self.es
        self.ident = es.enter_context(nc.sbuf_tensor("ident", [128, 128], F32))
        self.identb = es.enter_context(nc.sbuf_tensor("identb", [128, 128], BF16))
        self.triu = es.enter_context(nc.sbuf_tensor("triu", [128, 128], F32))
        self.ones = es.enter_context(nc.sbuf_tensor("ones", [128, 128], F32))
        self.onesb = es.enter_context(nc.sbuf_tensor("onesb", [128, 2], BF16))
        self.epsc = es.enter_context(nc.sbuf_tensor("epsc", [128, 1], F32))
        self.flag = es.enter_context(nc.sbuf_tensor("flag_sb", [128, 1], F32))
        self.nl16 = es.enter_context(nc.sbuf_tensor("nl16", [128, 1], F32))
        self.b_const = Buf("const")
        self.b_flag = Buf("flag")
        self.b_modd = Buf("modd")
        self.b_xfer = Buf("xfer")
        ARENA = 196 * 1024
        self.arena_t = es.enter_context(nc.sbuf_tensor("arena", [128, ARENA], U8))
        self.A = Arena(self.arena_t, ARENA)
        self.psum = [es.enter_context(nc.psum_tensor(f"ps{i}", [128, 1024], F32)) for i in range(4)]
        self.pbuf = [[Buf(f"ps{i}_{h}") for h in range(2)] for i in range(4)]
        self.b_PA1 = [self.pbuf[0][1], self.pbuf[0][1]]
        P = self.P
        cb = [self.b_const]
        P.op("pool", lambda e: e.memset(self.ident[:], 0.0), writes=cb)
        P.op("pool", lambda e: e.affine_select(out=self.ident[:], in_=self.ident[:], pattern=[[-1, 128]],
                                               compare_op=ALU.not_equal, fill=1.0, base=0, channel_multiplier=1),
             reads=cb, writes=cb)
        P.op("pool", lambda e: e.tensor_copy(self.identb[:], self.ident[:]), reads=cb, writes=cb)
        P.op("pool", lambda e: e.memset(self.ones[:], 1.0), writes=cb)
        P.op("pool", lambda e: e.affine_select(out=self.triu[:], in_=self.ones[:], pattern=[[1, 128]],
                                               compare_op=ALU.is_ge, fill=0.0, base=0, channel_multiplier=-1),
             reads=cb, writes=cb)
        P.op("pool", lambda e: e.memset(self.onesb[:], 1.0), writes=cb)
        P.op("pool", lambda e: e.memset(self.epsc[:], LN_EPS), writes=cb)
        P.op("pool", lambda e: e.memset(self.nl16[:], -float(np.log(16.0))), writes=cb)

    def din(self, name, shape, dt=F32):
        return self.nc.dram_tensor(name, list(shape), dt, kind="ExternalInput").ap()

    def dout(self, name, shape, dt=F32):
        return self.nc.dram_tensor(name, list(shape), dt, kind="ExternalOutput").ap()

    def dint(self, name, shape, dt=F32):
        return self.nc.dram_tensor(name, list(shape), dt, kind="Internal").ap()

    def mm(self, out, lhsT, rhs, start, stop, reads, writes):
        return self.P.op("pe", lambda e: e.matmul(out, lhsT=lhsT, rhs=rhs, start=start, stop=stop),
                         reads=reads, writes=writes)

    def tr(self, out, in_, ident, reads, writes):
        return self.P.op("pe", lambda e: e.transpose(out, in_, ident), reads=reads, writes=writes)

    def act(self, out, in_, func, reads, writes, bias=None, scale=None):
        kw = {}
        if bias is not None:
            kw["bias"] = bias
        if scale is not None:
            kw["scale"] = scale
        return self.P.op("act", lambda e: e.activation(out, in_, func, **kw), reads=reads, writes=writes)

    def dve(self, fn, reads, writes):
        return self.P.op("dve", fn, reads=reads, writes=writes)

    def pool(self, fn, reads, writes):
        return self.P.op("pool", fn, reads=reads, writes=writes)

    def dma(self, q, out, in_, reads=(), writes=()):
        return self.P.op(q, lambda e: e.dma_start(out=out, in_=in_), reads=reads, writes=writes)

    def defer(self, q, out, in_, writes):
        self.deferred.append((q, out, in_, writes))

    def pump(self, n):
        for _ in range(min(n, len(self.deferred))):
            q, out, in_, writes = self.deferred.pop(0)
            self.dma(q, out, in_, writes=writes)

    def flush(self):
        self.pump(len(self.deferred))

    def load_flag(self, flag_d):
        self.dma("sp", self.flag[:], flag_d, writes=[self.b_flag])

    def finish(self):
        self.P.emit(final_wait_ops=self.outs)
        self.es.close()
        return self.nc

    def rows_to_cols(self, rows, R, cols, b_rows, b_cols, pbank, pb):
        for c in range(8):
            self.tr(pbank[:, c * R:(c + 1) * R], rows[0:R, c * 128:(c + 1) * 128], self.ident[0:R, 0:R],
                    reads=[b_rows, self.b_const], writes=[pb])
        self.dve(lambda e: e.tensor_copy(cols, pbank[:, 0:8 * R].rearrange("p (c r) -> p c r", c=8)),
                 reads=[pb], writes=[b_cols])


def phase_p0(k, c_d, w_ada_d, b_ada_d, modd, nl):
    A = k.A
    A.reset()
    crow = A.t([8, 128], F32)
    condT = A.t([128, 8], F32)
    wa = [A.t([128, 8, 512], F32) for _ in range(2)]
    brow = A.t([1, 6 * D], F32)
    mrow = A.t([1, 6 * D], F32)
    b_crow, b_cond, b_brow, b_mrow = Buf("crow"), Buf("condT"), Buf("brow"), Buf("mrow")
    b_wa = [Buf("wa0"), Buf("wa1")]
    ps = k.psum[0]
    pb = k.pbuf[0]
    k.dma("sp", crow, c_d[0].rearrange("(k p) -> k p", p=128), writes=[b_crow])
    k.tr(ps[:, 0:8], crow[0:8, 0:128], k.ident[0:8, 0:8], reads=[b_crow, k.b_const], writes=[pb[0]])
    k.act(condT, ps[:, 0:8], AF.Silu, reads=[pb[0]], writes=[b_cond])
    n = 0
    for l in range(nl):
        k.dma("sp", brow, b_ada_d[l:l + 1, :], writes=[b_brow])
        for j in range(12):
            s = n % 2
            n += 1
            k.dma("sp", wa[s], w_ada_d[l][:, j * 512:(j + 1) * 512].rearrange("(k p) n -> p k n", p=128),
                  writes=[b_wa[s]])
            pbank = ps[0:1, s * 512:(s + 1) * 512]
            for kk in range(8):
                k.mm(pbank, condT[:, kk:kk + 1], wa[s][:, kk, :], kk == 0, kk == 7,
                     reads=[b_cond, b_wa[s]], writes=[pb[s]])
            k.dve(lambda e, j=j, pbank=pbank: e.tensor_tensor(mrow[0:1, j * 512:(j + 1) * 512], pbank,
                                                              brow[0:1, j * 512:(j + 1) * 512], op=ALU.add),
                  reads=[pb[s], b_brow], writes=[b_mrow])
        o = k.dma("sp", modd[l:l + 1, :], mrow, reads=[b_mrow], writes=[k.b_modd])
        k.outs.append(o)
    k.P.barrier()


XF = 2048 + 8 + 48 + 16
ALPHA = 8.0 ** 0.25


def conv_mixer_weights(k, tag, w_in_l, w_out_l):
    W = {}
    W["win"] = k.dint(f"winb_{tag}", [20, 128, 4096], BF16)
    W["wg"] = k.dint(f"wgb_{tag}", [128, 64], BF16)
    W["wout"] = k.dint(f"woutb_{tag}", [128, 8192], BF16)
    W["b_win"] = [Buf(f"winb{i}") for i in range(20)]
    W["b_wg"] = Buf("wgb")
    W["b_wout"] = Buf("woutb")

    def srcv(c0, n):
        return w_in_l[:, c0:c0 + n].rearrange("(k p) n -> p k n", p=128)

    def dstv(blk):
        return W["win"][blk].rearrange("p (k n) -> p k n", k=8)

    for blk in range(8):
        k.defer("gq", dstv(blk), srcv(blk * 512, 512), writes=[W["b_win"][blk]])
    k.defer("gq", W["wg"].rearrange("p (k n) -> p k n", k=8), srcv(4096, 8), writes=[W["b_wg"]])
    for j in range(8):
        for i, base in enumerate((4104, 5128, 6152)):
            k.defer("gq", dstv(8 + j)[:, :, i * 128:(i + 1) * 128], srcv(base + j * 128, 128),
                  writes=[W["b_win"][8 + j]])
    for j in range(4):
        k.defer("gq", dstv(16 + j), srcv(7176 + j * 512, 512), writes=[W["b_win"][16 + j]])
    k.defer("gq", W["wout"].rearrange("p (k n) -> p k n", k=8),
          w_out_l.rearrange("(k p) n -> p k n", p=128), writes=[W["b_wout"]])
    return W


def phase_mixer(k, x_src, x_dst, mod_l, prm, W, xfer_in, xfer_out, b_in=None, b_out=None, prepass=False):
    A = k.A
    A.reset()
    P = k.P
    PA, PB, PC, PD = k.psum
    bPA, bPB, bPC, bPD = k.pbuf
    bPAh = [[bPA[0]], [bPA[1]]]
    xt = A.t([128, NSUB, D], F32)
    b_xt = Buf("xt")
    hT = A.t([128, 8, T], BF16)
    b_hT = [Buf(f"hT{c}") for c in range(8)]
    wsl = [A.t([128, 8, 512], BF16) for _ in range(3)]
    b_wsl = [Buf(f"wsl{i}") for i in range(3)]
    wg = A.t([128, 8, 8], BF16)
    b_wgs = Buf("wg")
    wo = A.t([128, 8, D], BF16)
    b_wo = Buf("wo")
    qT = A.t([128, 8, T], BF16)
    kT = A.t([128, 8, T], BF16)
    g1 = A.t([128, 8, T], BF16)
    m2 = A.t([128, 8, T], BF16)
    b_qT = [Buf(f"qT{c}") for c in range(8)]
    b_kT = [Buf(f"kT{c}") for c in range(8)]
    b_g1 = [Buf(f"g1{c}") for c in range(8)]
    b_m2 = [Buf(f"m2{c}") for c in range(8)]
    v = A.t([128, NSUB, D], BF16)
    b_v = [Buf(f"v{s}") for s in range(NSUB)]
    U = [A.t([128, T + 3], F32) for _ in range(2)]
    b_U = [Buf("U0"), Buf("U1")]
    acc = [A.t([128, T], F32) for _ in range(2)]
    b_acc = [Buf("acc0"), Buf("acc1")]
    ccs = [A.t([128, T], F32) for _ in range(2)]
    b_ccs = [Buf("ccs0"), Buf("ccs1")]
    sgt = [A.t([128, T], BF16) for _ in range(2)]
    b_sgt = [Buf("sgt0"), Buf("sgt1")]
    hnTok = A.t([128, NSUB, D], BF16)
    b_hn = [Buf(f"hn{s}") for s in range(NSUB)]
    mT = A.t([128, 8, T], BF16)
    b_mT = [Buf(f"mT{c}") for c in range(8)]
    ya = [A.t([128, T], BF16) for _ in range(2)]
    b_ya = [Buf("ya0"), Buf("ya1")]
    xf = A.t([128, XF], F32)
    Cst = xf[:, 0:2048].rearrange("p (j h d) -> p j h d", j=2, h=4)
    nS = xf[:, 2048:2056].rearrange("p (j h) -> p j h", j=2)
    haloU = xf[:, 2056:2104].rearrange("p (c t) -> p c t", c=16)
    haloU2 = xf[:, 2104:2120].rearrange("p (c t) -> p c t", c=8)
    b_C = [Buf(f"C{h}") for h in range(4)]
    b_nS = Buf("nS")
    b_hU = [Buf(f"hU{c}") for c in range(16)]
    b_hU2 = [Buf(f"hU2{c}") for c in range(8)]
    Cb = A.t([128, 2, 4, DH], BF16)
    b_Cb = [Buf(f"Cb{h}") for h in range(4)]
    nb = A.t([128, 2, 4], BF16)
    b_nb = Buf("nb")
    kS = [A.t([128, 4, DH], BF16) for _ in range(2)]
    b_kS = [Buf("kS0"), Buf("kS1")]
    Pm = [A.t([128, 4, 128], BF16) for _ in range(2)]
    b_Pm = [Buf("Pm0"), Buf("Pm1")]
    gates8 = A.t([128, NSUB, 8], F32)
    b_g8 = [Buf(f"g8{s}") for s in range(NSUB)]
    sm = [A.t([128, 96], F32) for _ in range(2)]
    b_sm = [Buf("sm0"), Buf("sm1")]
    rb = [A.t([128, D], F32) for _ in range(2)]
    b_rb = [Buf("rb0"), Buf("rb1")]
    G1 = A.t([128, D], F32)
    lng = A.t([128, D], F32)
    lnb = A.t([128, D], F32)
    bg = A.t([128, 8], F32)
    b_G1, b_lng, b_lnb, b_bg = Buf("G1"), Buf("lng"), Buf("lnb"), Buf("bg")
    rows = A.t([14, D], F32)
    b_rows = Buf("rows")
    cols = A.t([128, 8, 14], F32)
    b_cols = Buf("cols")
    sc1 = A.t([128, 8], F32)
    b_sc1 = Buf("sc1")
    cb = [k.b_const]

    k.dma("sp", rows[0:1, :], mod_l[:, 0:D], reads=[k.b_modd], writes=[b_rows])
    k.dma("sp", rows[1:2, :], mod_l[:, D:2 * D], reads=[k.b_modd], writes=[b_rows])
    k.dma("sp", rows[2:3, :], prm["hn_gain"], writes=[b_rows])
    k.dma("sp", rows[3:6, :], prm["conv_short"], writes=[b_rows])
    k.dma("sp", rows[6:10, :], prm["conv_qk"][:, 0:D], writes=[b_rows])
    k.dma("sp", rows[10:14, :], prm["conv_qk"][:, D:2 * D], writes=[b_rows])
    k.rows_to_cols(rows, 14, cols, b_rows, b_cols, PA[:, 0:512], bPA[0])
    k.dve(lambda e: e.tensor_scalar_add(sc1, cols[:, :, 1], 1.0), reads=[b_cols], writes=[b_sc1])
    k.dma("sp", G1, mod_l[:, 2 * D:3 * D].partition_broadcast(128), reads=[k.b_modd], writes=[b_G1])
    k.pool(lambda e: e.tensor_scalar_add(G1, G1, 1.0), reads=[b_G1], writes=[b_G1])
    k.dma("sp", lng, prm["ln_g"].partition_broadcast(128), writes=[b_lng])
    k.dma("sp", lnb, prm["ln_b"].partition_broadcast(128), writes=[b_lnb])
    k.dma("sp", bg, prm["b_gates"].partition_broadcast(128), writes=[b_bg])
    if not prepass:
        k.dma("sp", wo, W["wout"].rearrange("p (k n) -> p k n", k=8), reads=[W["b_wout"]], writes=[b_wo])
    k.dma("sp", wg, W["wg"].rearrange("p (k n) -> p k n", k=8), reads=[W["b_wg"]], writes=[b_wgs])
    b_xf_all = b_C + [b_nS] + b_hU + b_hU2
    if xfer_in is None:
        k.dve(lambda e: e.memset(xf, 0.0), reads=[], writes=b_xf_all)
    else:
        k.dma("sp", xf, xfer_in, reads=[b_in or k.b_xfer], writes=b_xf_all)
        k.dve(lambda e: e.tensor_scalar_mul(xf, xf, k.flag[:, 0:1]), reads=b_xf_all + [k.b_flag], writes=b_xf_all)
    for h in range(4):
        k.act(Cb[:, :, h, :], Cst[:, :, h, :], AF.Copy, reads=[b_C[h]], writes=[b_Cb[h]])
    k.act(nb, nS, AF.Copy, reads=[b_nS], writes=[b_nb])

    banks = [(PB[:, 0:512], bPB[0]), (PB[:, 512:1024], bPB[1]), (PC[:, 0:512], bPC[0]), (PC[:, 512:1024], bPC[1])]
    st = {"bank": 0, "w": 0, "u": 0}

    def next_bank():
        b = banks[st["bank"] % 4]
        st["bank"] += 1
        return b

    def load_w(blk, ncols=512):
        i = st["w"] % 3
        st["w"] += 1
        k.dma("sp", wsl[i][:, :, 0:ncols], W["win"][blk].rearrange("p (k n) -> p k n", k=8)[:, :, 0:ncols],
              reads=[W["b_win"][blk]], writes=[b_wsl[i]])
        return wsl[i], b_wsl[i]

    def proj_fm(wt, bw, c0):
        pbank, pb = next_bank()
        for kk in range(8):
            k.mm(pbank, wt[:, kk, c0:c0 + 128], hT[:, kk, :], kk == 0, kk == 7,
                 reads=[bw, b_hT[kk]], writes=[pb])
        return pbank, pb

    LN16 = float(np.log(16.0))
    pden = PD[:, 16:20]
    pbp = PD[:, 8:16]
    pdn = PD[:, 20:28].rearrange("p (j h) -> p j h", j=2)
    b_pg = [bPD[0]] * NSUB
    b_pbp = b_pden = b_pdn = bPD[0]
    PD1b = PD[:, 512:1024].bitcast(BF16)
    PA1b = PA[:, 512:1024].bitcast(BF16)
    b_PA1b = k.b_PA1

    for ti in range(k.NTILE):
        t0 = ti * T
        k.pump(k.pump_rate)
        k.dma("sp", xt, x_src[t0:t0 + T, :].rearrange("(s p) d -> p s d", p=128), writes=[b_xt])
        for c in range(8):
            hb = c % 2
            pbank = PA[:, hb * 512:(hb + 1) * 512]
            for s in range(NSUB):
                k.tr(pbank[:, s * 128:(s + 1) * 128], xt[:, s, c * 128:(c + 1) * 128], k.ident[:],
                     reads=[b_xt, k.b_const], writes=bPAh[hb])
            k.act(hT[:, c, :], pbank, AF.Identity, reads=bPAh[hb] + [b_sc1, b_cols], writes=[b_hT[c]],
                  scale=sc1[:, c:c + 1], bias=cols[:, c, 0:1])
        k.pool(lambda e: e.tensor_scalar_mul(xt, xt, ALPHA), reads=[b_xt], writes=[b_xt])
        last = ti == k.NTILE - 1
        for blk in range(4):
            if prepass and blk < 2:
                if last:
                    wt, bw = load_w(blk)
                    for cc in range(4):
                        c16 = blk * 4 + cc
                        pbank, pb = next_bank()
                        for kk in range(8):
                            k.mm(pbank[:, 0:3], wt[:, kk, cc * 128:(cc + 1) * 128], hT[:, kk, T - 3:T], kk == 0, kk == 7,
                                 reads=[bw, b_hT[kk]], writes=[pb])
                        k.dve(lambda e, c16=c16, pbank=pbank: e.tensor_copy(haloU[:, c16, :], pbank[:, 0:3]),
                              reads=[pb], writes=[b_hU[c16]])
                continue
            wt, bw = load_w(blk)
            for cc in range(4):
                c16 = blk * 4 + cc
                c8 = c16 % 8
                rbase = 6 if c16 < 8 else 10
                dstT, b_dst = (qT, b_qT) if c16 < 8 else (kT, b_kT)
                pbank, pb = proj_fm(wt, bw, cc * 128)
                u = st["u"] % 2
                st["u"] += 1
                Ut, bU, ac, bac = U[u], b_U[u], acc[u], b_acc[u]
                k.pool(lambda e, Ut=Ut, c16=c16: e.tensor_copy(Ut[:, 0:3], haloU[:, c16, :]),
                       reads=[b_hU[c16]], writes=[bU])
                k.act(Ut[:, 3:T + 3], pbank, AF.Copy, reads=[pb], writes=[bU])
                k.pool(lambda e, Ut=Ut, c16=c16: e.tensor_copy(haloU[:, c16, :], Ut[:, T:T + 3]),
                       reads=[bU], writes=[b_hU[c16]])
                k.pool(lambda e, Ut=Ut, ac=ac, c8=c8, rbase=rbase: e.tensor_scalar_mul(
                    ac, Ut[:, 0:T], cols[:, c8, rbase:rbase + 1]), reads=[bU, b_cols], writes=[bac])
                for j in range(1, 4):
                    k.dve(lambda e, Ut=Ut, ac=ac, c8=c8, rbase=rbase, j=j: e.scalar_tensor_tensor(
                        ac, in0=Ut[:, j:j + T], scalar=cols[:, c8, rbase + j:rbase + j + 1], in1=ac,
                        op0=ALU.mult, op1=ALU.add), reads=[bU, b_cols, bac], writes=[bac])
                k.act(dstT[:, c8, :], ac, AF.Silu, reads=[bac], writes=[b_dst[c8]])
        for blk in (4, 5):
            wt, bw = load_w(blk)
            for s in range(NSUB):
                pbank, pb = next_bank()
                for kk in range(8):
                    k.mm(pbank, hT[:, kk, s * 128:(s + 1) * 128], wt[:, kk, :], kk == 0, kk == 7,
                         reads=[bw, b_hT[kk]], writes=[pb])
                k.act(v[:, s, (blk - 4) * 512:(blk - 3) * 512], pbank, AF.Copy, reads=[pb], writes=[b_v[s]])
        for blk in (() if prepass else (6, 7)):
            wt, bw = load_w(blk)
            for cc in range(4):
                c8 = (blk - 6) * 4 + cc
                pbank, pb = proj_fm(wt, bw, cc * 128)
                k.act(g1[:, c8, :], pbank, AF.Sigmoid, reads=[pb], writes=[b_g1[c8]])
        for s in range(NSUB):
            pg = PD[:, 32 + s * 8:32 + (s + 1) * 8]
            for kk in range(8):
                k.mm(pg, hT[:, kk, s * 128:(s + 1) * 128], wg[:, kk, :], kk == 0, kk == 7,
                     reads=[b_wgs, b_hT[kk]], writes=[b_pg[s]])
            k.dve(lambda e, s=s, pg=pg: e.tensor_tensor(gates8[:, s, :], pg, bg, op=ALU.add),
                  reads=[b_pg[s], b_bg], writes=[b_g8[s]])
        for j in range(8):
            if prepass:
                if last:
                    wt, bw = load_w(8 + j, 384)
                    pcc, pbcc = next_bank()
                    pcx, pbcx = next_bank()
                    for (pbk, pbb, c0) in ((pcc, pbcc, 128), (pcx, pbcx, 256)):
                        for kk in range(8):
                            k.mm(pbk[:, 0:2], wt[:, kk, c0:c0 + 128], hT[:, kk, T - 2:T], kk == 0, kk == 7,
                                 reads=[bw, b_hT[kk]], writes=[pbb])
                    u = st["u"] % 2
                    st["u"] += 1
                    k.act(ccs[u][:, 0:2], pcc[:, 0:2], AF.Copy, reads=[pbcc], writes=[b_ccs[u]])
                    k.dve(lambda e, j=j, u=u, pcx=pcx: e.tensor_tensor(haloU2[:, j, :], pcx[:, 0:2], ccs[u][:, 0:2], op=ALU.mult),
                          reads=[pbcx, b_ccs[u]], writes=[b_hU2[j]])
                continue
            wt, bw = load_w(8 + j, 384)
            pcb, pbcb = proj_fm(wt, bw, 0)
            pcc, pbcc = proj_fm(wt, bw, 128)
            pcx, pbcx = proj_fm(wt, bw, 256)
            u = st["u"] % 2
            st["u"] += 1
            Ut, bU, ac, bac, cs, bcs = U[u], b_U[u], acc[u], b_acc[u], ccs[u], b_ccs[u]
            k.act(cs, pcc, AF.Copy, reads=[pbcc], writes=[bcs])
            k.pool(lambda e, Ut=Ut, j=j: e.tensor_copy(Ut[:, 0:2], haloU2[:, j, :]), reads=[b_hU2[j]], writes=[bU])
            k.dve(lambda e, Ut=Ut, cs=cs, pcx=pcx: e.tensor_tensor(Ut[:, 2:T + 2], pcx, cs, op=ALU.mult),
                  reads=[pbcx, bcs], writes=[bU])
            k.pool(lambda e, Ut=Ut, j=j: e.tensor_copy(haloU2[:, j, :], Ut[:, T:T + 2]), reads=[bU], writes=[b_hU2[j]])
            k.pool(lambda e, Ut=Ut, ac=ac, j=j: e.tensor_scalar_mul(ac, Ut[:, 0:T], cols[:, j, 3:4]),
                   reads=[bU, b_cols], writes=[bac])
            for tp in (1, 2):
                k.dve(lambda e, Ut=Ut, ac=ac, j=j, tp=tp: e.scalar_tensor_tensor(
                    ac, in0=Ut[:, tp:tp + T], scalar=cols[:, j, 3 + tp:4 + tp], in1=ac, op0=ALU.mult, op1=ALU.add),
                    reads=[bU, b_cols, bac], writes=[bac])
            k.dve(lambda e, ac=ac, pcb=pcb, j=j: e.tensor_tensor(m2[:, j, :], ac, pcb, op=ALU.mult),
                  reads=[bac, pbcb], writes=[b_m2[j]])
        for blk in (() if prepass else range(16, 20)):
            wt, bw = load_w(blk)
            for cc in range(4):
                c8 = ((blk - 16) % 2) * 4 + cc
                tgt, b_tgt = (g1, b_g1) if blk < 18 else (m2, b_m2)
                pbank, pb = proj_fm(wt, bw, cc * 128)
                u = st["u"] % 2
                st["u"] += 1
                k.act(sgt[u], pbank, AF.Sigmoid, reads=[pb], writes=[b_sgt[u]])
                k.pool(lambda e, tgt=tgt, c8=c8, u=u: e.tensor_tensor(tgt[:, c8, :], tgt[:, c8, :], sgt[u], op=ALU.mult),
                       reads=[b_tgt[c8], b_sgt[u]], writes=[b_tgt[c8]])

        for s in range(NSUB):
            sub = slice(s * 128, (s + 1) * 128)
            z = (ti * NSUB + s) % 2
            S = sm[z]
            bS = b_sm[z]
            e1, l1, tmp4, ws_, ebt, ebL = S[:, 0:4], S[:, 4:8], S[:, 8:12], S[:, 12:16], S[:, 16:20], S[:, 20:24]
            d1, d2, scl, t1, t3, aa = S[:, 24:28], S[:, 28:32], S[:, 32:36], S[:, 36:40], S[:, 40:44], S[:, 44:48]
            stt = S[:, 48:72].rearrange("p (h x) -> p h x", h=4)
            mv = S[:, 72:80].rearrange("p (h x) -> p h x", h=4)
            k.act(e1, gates8[:, s, 4:8], AF.Exp, reads=[b_g8[s]], writes=[bS], scale=-1.0)
            k.act(l1, e1, AF.Ln, reads=[bS], writes=[bS], bias=1.0)
            k.mm(pbp[:, 0:4], k.triu[:], l1, True, True, reads=[bS, k.b_const], writes=[b_pbp])
            k.mm(pbp[:, 4:8], k.ones[:], l1, True, True, reads=[bS, k.b_const], writes=[b_pbp])
            k.dve(lambda e, tmp4=tmp4, s=s: e.tensor_tensor(tmp4, gates8[:, s, 0:4], pbp[:, 0:4], op=ALU.add),
                  reads=[b_g8[s], b_pbp], writes=[bS])
            k.act(ws_, tmp4, AF.Exp, reads=[bS], writes=[bS], bias=k.nl16[:, 0:1])
            if not prepass:
                k.act(ebt, pbp[:, 0:4], AF.Exp, reads=[b_pbp], writes=[bS], scale=-1.0)
            k.act(ebL, pbp[:, 4:8], AF.Exp, reads=[b_pbp], writes=[bS], scale=-1.0)
            for h in range(4):
                for j in range(2):
                    k.tr(PD1b[:, (h * 2 + j) * 128:(h * 2 + j + 1) * 128], kT[:, 2 * h + j, sub], k.identb[:],
                         reads=[b_kT[2 * h + j], k.b_const], writes=[bPD[1]])
            for h in range(4):
                k.dve(lambda e, h=h, z=z, ws_=ws_: e.tensor_scalar_mul(kS[z][:, h, :], PD1b[:, h * 256:(h + 1) * 256],
                                                                       ws_[:, h:h + 1]),
                      reads=[bPD[1], bS], writes=[b_kS[z]])
            for h in (() if prepass else range(4)):
                for j in range(2):
                    k.mm(PA[:, h * 128:(h + 1) * 128], kT[:, 2 * h + j, sub], qT[:, 2 * h + j, sub], j == 0, j == 1,
                         reads=[b_kT[2 * h + j], b_qT[2 * h + j]], writes=[bPA[0]])
            for h in (() if prepass else range(4)):
                k.dve(lambda e, h=h, z=z, ws_=ws_: e.scalar_tensor_tensor(
                    Pm[z][:, h, :], in0=PA[:, h * 128:(h + 1) * 128], scalar=ws_[:, h:h + 1], in1=k.triu[:],
                    op0=ALU.mult, op1=ALU.mult), reads=[bPA[0], bS, k.b_const], writes=[b_Pm[z]])
            for h in (() if prepass else range(4)):
                hb = h // 2
                nump = PB[:, h * 256:(h + 1) * 256]
                k.mm(nump, Pm[z][:, h, :], v[:, s, h * 256:(h + 1) * 256], True, False,
                     reads=[b_Pm[z], b_v[s]], writes=[bPB[hb]])
                for j in range(2):
                    k.mm(nump, qT[:, 2 * h + j, sub], Cb[:, j, h, :], False, j == 1,
                         reads=[b_qT[2 * h + j], b_Cb[h]], writes=[bPB[hb]])
                k.mm(pden[:, h:h + 1], Pm[z][:, h, :], k.onesb[:, 0:1], True, False,
                     reads=[b_Pm[z], k.b_const], writes=[b_pden])
                for j in range(2):
                    k.mm(pden[:, h:h + 1], qT[:, 2 * h + j, sub], nb[:, j, h:h + 1], False, j == 1,
                         reads=[b_qT[2 * h + j], b_nb], writes=[b_pden])
            for h in range(4):
                hb = h % 2
                pdel = PC[:, hb * 512:(hb + 1) * 512]
                for j in range(2):
                    k.mm(pdel[:, j * 256:(j + 1) * 256], kS[z][:, h, j * 128:(j + 1) * 128],
                         v[:, s, h * 256:(h + 1) * 256], True, True, reads=[b_kS[z], b_v[s]], writes=[bPC[hb]])
                    k.mm(pdn[:, j, h:h + 1], kS[z][:, h, j * 128:(j + 1) * 128], k.onesb[:, 0:1], True, True,
                         reads=[b_kS[z], k.b_const], writes=[b_pdn])
                k.pool(lambda e, h=h, ebL=ebL: e.tensor_scalar_mul(Cst[:, :, h, :], Cst[:, :, h, :], ebL[:, h:h + 1]),
                       reads=[b_C[h], bS], writes=[b_C[h]])
                k.dve(lambda e, h=h, ebL=ebL, pdel=pdel: e.scalar_tensor_tensor(
                    Cst[:, :, h, :], in0=pdel.rearrange("p (j d) -> p j d", j=2), scalar=ebL[:, h:h + 1],
                    in1=Cst[:, :, h, :], op0=ALU.mult, op1=ALU.add), reads=[bPC[hb], bS, b_C[h]], writes=[b_C[h]])
                if not prepass:
                    k.act(Cb[:, :, h, :], Cst[:, :, h, :], AF.Copy, reads=[b_C[h]], writes=[b_Cb[h]])
            k.dve(lambda e: e.tensor_tensor(nS, nS, pdn, op=ALU.add), reads=[b_nS, b_pdn], writes=[b_nS])
            k.dve(lambda e, ebL=ebL: e.tensor_tensor(nS, nS, ebL.unsqueeze(1).to_broadcast([128, 2, 4]), op=ALU.mult),
                  reads=[b_nS, bS], writes=[b_nS])
            if prepass:
                continue
            k.act(nb, nS, AF.Copy, reads=[b_nS], writes=[b_nb])
            k.dve(lambda e, d1=d1, ebt=ebt: e.tensor_tensor(d1, pden, ebt, op=ALU.mult), reads=[b_pden, bS], writes=[bS])
            k.act(d2, d1, AF.Abs, reads=[bS], writes=[bS])
            k.dve(lambda e, d2=d2: e.tensor_scalar_max(d2, d2, 1.0), reads=[bS], writes=[bS])
            k.dve(lambda e, d2=d2: e.reciprocal(d2, d2), reads=[bS], writes=[bS])
            k.dve(lambda e, d2=d2, scl=scl, ebt=ebt: e.tensor_tensor(scl, ebt, d2, op=ALU.mult), reads=[bS], writes=[bS])
            for h in range(4):
                k.dve(lambda e, h=h, stt=stt: e.bn_stats(stt[:, h, :], PB[:, h * 256:(h + 1) * 256]),
                      reads=[bPB[h // 2]], writes=[bS])
                k.dve(lambda e, h=h, stt=stt, mv=mv: e.bn_aggr(mv[:, h, :], stt[:, h, :]), reads=[bS], writes=[bS])
            k.dve(lambda e, t1=t1, scl=scl: e.tensor_tensor(t1, scl, scl, op=ALU.mult), reads=[bS], writes=[bS])
            k.dve(lambda e, t1=t1, mv=mv: e.tensor_tensor(t1, t1, mv[:, :, 1], op=ALU.mult), reads=[bS], writes=[bS])
            k.act(t3, t1, AF.Sqrt, reads=[bS, k.b_const], writes=[bS], bias=k.epsc[:, 0:1])
            k.dve(lambda e, t3=t3: e.reciprocal(t3, t3), reads=[bS], writes=[bS])
            k.dve(lambda e, t3=t3, aa=aa, scl=scl: e.tensor_tensor(aa, t3, scl, op=ALU.mult), reads=[bS], writes=[bS])
            for h in range(4):
                k.dve(lambda e, h=h, s=s, mv=mv, aa=aa: e.tensor_scalar(
                    hnTok[:, s, h * 256:(h + 1) * 256], PB[:, h * 256:(h + 1) * 256], mv[:, h, 0:1], aa[:, h:h + 1],
                    op0=ALU.subtract, op1=ALU.mult), reads=[bPB[h // 2], bS], writes=[b_hn[s]])

        if prepass:
            continue
        for c in range(8):
            hb = c % 2
            for s in range(NSUB):
                k.tr(PA1b[:, hb * 512 + s * 128:hb * 512 + (s + 1) * 128], hnTok[:, s, c * 128:(c + 1) * 128], k.identb[:],
                     reads=[b_hn[s], k.b_const], writes=[b_PA1b[hb]])
            k.dve(lambda e, c=c, hb=hb: e.scalar_tensor_tensor(
                ya[hb], in0=PA1b[:, hb * 512:(hb + 1) * 512], scalar=cols[:, c, 2:3], in1=g1[:, c, :],
                op0=ALU.mult, op1=ALU.mult), reads=[b_PA1b[hb], b_cols, b_g1[c]], writes=[b_ya[hb]])
            k.pool(lambda e, c=c, hb=hb: e.tensor_tensor(mT[:, c, :], ya[hb], m2[:, c, :], op=ALU.add),
                   reads=[b_ya[hb], b_m2[c]], writes=[b_mT[c]])

        for s in range(NSUB):
            sub = slice(s * 128, (s + 1) * 128)
            yb_, byb = (PB, bPB) if s % 2 == 0 else (PC, bPC)
            z = s % 2
            for n in range(2):
                for c in range(8):
                    k.mm(yb_[:, n * 512:(n + 1) * 512], mT[:, c, sub], wo[:, c, n * 512:(n + 1) * 512], c == 0, c == 7,
                         reads=[b_mT[c], b_wo], writes=[byb[n]])
            ln_epilogue(k, yb_[:], byb, rb[z], b_rb[z], xt[:, s, :], b_xt, G1, b_G1, lng, b_lng, lnb, b_lnb,
                        sm[z][:, 80:96], b_sm[z], x_dst[t0 + s * 128:t0 + (s + 1) * 128, :])
    if xfer_out is not None:
        o = k.dma("sp", xfer_out, xf, reads=b_xf_all, writes=[b_out or k.b_xfer])
        k.outs.append(o)
    P.barrier()


def ln_epilogue(k, yps, byps, r, b_r, xres, b_xres, G, b_G, lng, b_lng, lnb, b_lnb, S, bS, dst):
    st6 = S[:, 0:12].rearrange("p (a b) -> p a b", a=2)
    mv = S[:, 12:14]
    sq = S[:, 14:15]
    nmr = S[:, 15:16]
    if yps is not None:
        k.dve(lambda e: e.tensor_tensor(r, yps, G, op=ALU.mult), reads=byps + [b_G], writes=[b_r])
    k.pool(lambda e: e.tensor_tensor(r, r, xres, op=ALU.add), reads=[b_r, b_xres], writes=[b_r])
    for a in range(2):
        k.dve(lambda e, a=a: e.bn_stats(st6[:, a, :], r[:, a * 512:(a + 1) * 512]), reads=[b_r], writes=[bS])
    k.dve(lambda e: e.bn_aggr(mv, S[:, 0:12]), reads=[bS], writes=[bS])
    k.act(sq, mv[:, 1:2], AF.Sqrt, reads=[bS, k.b_const], writes=[bS], bias=k.epsc[:, 0:1])
    k.dve(lambda e: e.reciprocal(sq, sq), reads=[bS], writes=[bS])
    k.dve(lambda e: e.tensor_scalar(nmr, mv[:, 0:1], -1.0, sq, op0=ALU.mult, op1=ALU.mult), reads=[bS], writes=[bS])
    k.act(r, r, AF.Identity, reads=[b_r, bS], writes=[b_r], scale=sq, bias=nmr)
    k.pool(lambda e: e.tensor_tensor(r, r, lng, op=ALU.mult), reads=[b_r, b_lng], writes=[b_r])
    k.pool(lambda e: e.tensor_tensor(r, r, lnb, op=ALU.add), reads=[b_r, b_lnb], writes=[b_r])
    o = k.dma("sp", dst, r, reads=[b_r])
    k.outs.append(o)
    return o


def conv_ffn_weights(k, tag, w13_l, w2_l):
    W = {}
    W["w13"] = k.dint(f"w13b_{tag}", [14, 128, 8 * 2 * 256], BF16)
    W["w2"] = k.dint(f"w2b_{tag}", [2, 128, NF * 512], BF16)
    W["b_w13"] = [Buf(f"w13b{g}") for g in range(14)]
    W["b_w2"] = [Buf("w2b0"), Buf("w2b1")]
    for g in range(14):
        dv = W["w13"][g].rearrange("p (k i n) -> p k i n", k=8, i=2)
        for i in range(2):
            k.defer("gq", dv[:, :, i, :], w13_l[:, i * DFF + g * 256:i * DFF + (g + 1) * 256].rearrange(
                "(k p) n -> p k n", p=128), writes=[W["b_w13"][g]])
    for n in range(2):
        dv = W["w2"][n].rearrange("p (f c) -> p f c", f=NF)
        for q in range(4):
            k.defer("gq", dv[:, q * 7:(q + 1) * 7, :],
                  w2_l[q * 7 * 128:(q + 1) * 7 * 128, n * 512:(n + 1) * 512].rearrange("(f p) c -> p f c", p=128),
                  writes=[W["b_w2"][n]])
    return W


def phase_ffn(k, x_src, x_dst, mod_l, prm, experts, router_d, final=False):
    A = k.A
    A.reset()
    P = k.P
    PA, PB, PC, PD = k.psum
    bPA, bPB, bPC, bPD = k.pbuf
    bPAh = [[bPA[0]], [bPA[1]]]
    moe = router_d is not None
    xt = A.t([128, NSUB, D], F32)
    b_xt = Buf("xt")
    hT = A.t([128, 8, T], BF16)
    b_hT = [Buf(f"hT{c}") for c in range(8)]
    g = A.t([128, NF, T], BF16)
    b_g = [Buf(f"g{f}") for f in range(NF)]
    yacc = A.t([128, NSUB, D], F32)
    b_y = [Buf(f"y{s}") for s in range(NSUB)]
    w13s = [A.t([128, 8, 2, 256], BF16) for _ in range(2)]
    b_w13s = [Buf("w13s0"), Buf("w13s1")]
    w2s = [A.t([128, NF, 512], BF16) for _ in range(2)]
    b_w2s = [Buf("w2s0"), Buf("w2s1")]
    G2 = A.t([128, D], F32)
    lng = A.t([128, D], F32)
    lnb = A.t([128, D], F32)
    b_G2, b_lng, b_lnb = Buf("G2"), Buf("lng"), Buf("lnb")
    sa = [A.t([128, T], F32) for _ in range(2)]
    b_sa = [Buf("sa0"), Buf("sa1")]
    rows = A.t([2, D], F32)
    b_rows = Buf("rows")
    cols = A.t([128, 8, 2], F32)
    b_cols = Buf("cols")
    sc1 = A.t([128, 8], F32)
    b_sc1 = Buf("sc1")
    sm = [A.t([128, 16], F32) for _ in range(2)]
    b_sm = [Buf("sm0"), Buf("sm1")]
    if moe:
        hT32 = A.t([128, 8, T], F32)
        b_hT32 = [Buf(f"hT32{c}") for c in range(8)]
        wr = A.t([128, 8, NE], F32)
        b_wr = Buf("wr")
        gates = A.t([128, NSUB, NE], F32)
        b_gates = [Buf(f"gates{s}") for s in range(NSUB)]
        rt = [A.t([128, 64], F32) for _ in range(2)]
        b_rt = [Buf("rt0"), Buf("rt1")]

    k.dma("sp", rows[0:1, :], mod_l[:, 3 * D:4 * D], reads=[k.b_modd], writes=[b_rows])
    k.dma("sp", rows[1:2, :], mod_l[:, 4 * D:5 * D], reads=[k.b_modd], writes=[b_rows])
    k.rows_to_cols(rows, 2, cols, b_rows, b_cols, PA[:, 0:512], bPA[0])
    k.dve(lambda e: e.tensor_scalar_add(sc1, cols[:, :, 1], 1.0), reads=[b_cols], writes=[b_sc1])
    k.dma("sp", G2, mod_l[:, 5 * D:6 * D].partition_broadcast(128), reads=[k.b_modd], writes=[b_G2])
    k.pool(lambda e: e.tensor_scalar_add(G2, G2, 1.0), reads=[b_G2], writes=[b_G2])
    k.dma("sp", lng, prm["ln_g"].partition_broadcast(128), writes=[b_lng])
    k.dma("sp", lnb, prm["ln_b"].partition_broadcast(128), writes=[b_lnb])
    if moe:
        k.dma("sp", wr, router_d.rearrange("(k p) e -> p k e", p=128), writes=[b_wr])

    banks = [(PB[:, 0:512], bPB[0]), (PB[:, 512:1024], bPB[1]), (PC[:, 0:512], bPC[0]), (PC[:, 512:1024], bPC[1])]
    st = {"bank": 0, "w13": 0, "w2": 0, "u": 0}

    def next_bank():
        b = banks[st["bank"] % 4]
        st["bank"] += 1
        return b

    for ti in range(k.NTILE):
        t0 = ti * T
        k.pump(k.pump_rate)
        k.dma("sp", xt, x_src[t0:t0 + T, :].rearrange("(s p) d -> p s d", p=128), writes=[b_xt])
        for c in range(8):
            hb = c % 2
            pbank = PA[:, hb * 512:(hb + 1) * 512]
            for s in range(NSUB):
                k.tr(pbank[:, s * 128:(s + 1) * 128], xt[:, s, c * 128:(c + 1) * 128], k.ident[:],
                     reads=[b_xt, k.b_const], writes=bPAh[hb])
            k.act(hT[:, c, :], pbank, AF.Identity, reads=bPAh[hb] + [b_sc1, b_cols], writes=[b_hT[c]],
                  scale=sc1[:, c:c + 1], bias=cols[:, c, 0:1])
            if moe and "H" not in _DBG:
                k.act(hT32[:, c, :], pbank, AF.Identity, reads=bPAh[hb] + [b_sc1, b_cols], writes=[b_hT32[c]],
                      scale=sc1[:, c:c + 1], bias=cols[:, c, 0:1])
        k.pool(lambda e: e.tensor_scalar_mul(xt, xt, ALPHA), reads=[b_xt], writes=[b_xt])
        if moe and "R" in _DBG:
            for s in range(NSUB):
                k.dve(lambda e, s=s: e.memset(gates[:, s, :], 0.125), reads=[], writes=[b_gates[s]])
        elif moe:
            for s in range(NSUB):
                z = s % 2
                R = rt[z]
                bR = b_rt[z]
                lg, mx8, m1k, m2k, dd, p1, p2 = (R[:, 0:8], R[:, 8:16], R[:, 16:24], R[:, 24:32], R[:, 32:33],
                                                 R[:, 33:34], R[:, 34:35])
                pl = PD[:, s * 8:(s + 1) * 8]
                if "M" in _DBG:
                    k.dve(lambda e, lg=lg, s=s: e.tensor_copy(lg, hT32[:, 0, s * 8:(s + 1) * 8]), reads=[b_hT32[0]], writes=[bR])
                else:
                    for kk in range(8):
                        k.mm(pl, hT32[:, kk, s * 128:(s + 1) * 128], wr[:, kk, :], kk == 0, kk == 7,
                             reads=[b_hT32[kk], b_wr], writes=[bPD[0]])
                    k.dve(lambda e, lg=lg, pl=pl: e.tensor_copy(lg, pl), reads=[bPD[0]], writes=[bR])
                k.dve(lambda e, lg=lg, mx8=mx8: e.max(mx8, lg), reads=[bR], writes=[bR])
                k.dve(lambda e, lg=lg, mx8=mx8, m1k=m1k: e.tensor_scalar(m1k, lg, mx8[:, 0:1], None, op0=ALU.is_equal),
                      reads=[bR], writes=[bR])
                k.dve(lambda e, lg=lg, mx8=mx8, m2k=m2k: e.tensor_scalar(m2k, lg, mx8[:, 1:2], None, op0=ALU.is_equal),
                      reads=[bR], writes=[bR])
                k.dve(lambda e, mx8=mx8, dd=dd: e.tensor_tensor(dd, mx8[:, 1:2], mx8[:, 0:1], op=ALU.subtract),
                      reads=[bR], writes=[bR])
                k.act(p2, dd, AF.Exp, reads=[bR], writes=[bR])
                k.dve(lambda e, p1=p1, p2=p2: e.tensor_scalar_add(p1, p2, 1.0), reads=[bR], writes=[bR])
                k.dve(lambda e, p1=p1: e.reciprocal(p1, p1), reads=[bR], writes=[bR])
                k.dve(lambda e, p1=p1, p2=p2: e.tensor_tensor(p2, p2, p1, op=ALU.mult), reads=[bR], writes=[bR])
                k.dve(lambda e, s=s, m1k=m1k, p1=p1: e.tensor_scalar_mul(gates[:, s, :], m1k, p1),
                      reads=[bR], writes=[b_gates[s]])
                k.dve(lambda e, s=s, m2k=m2k, p2=p2: e.scalar_tensor_tensor(
                    gates[:, s, :], in0=m2k, scalar=p2, in1=gates[:, s, :], op0=ALU.mult, op1=ALU.add),
                    reads=[bR, b_gates[s]], writes=[b_gates[s]])
        for ei, We in enumerate(experts):
            for gi in range(14):
                i = st["w13"] % 2
                st["w13"] += 1
                k.dma("sp", w13s[i], We["w13"][gi].rearrange("p (k i n) -> p k i n", k=8, i=2),
                      reads=[We["b_w13"][gi]], writes=[b_w13s[i]])
                for i2 in range(2):
                    f = gi * 2 + i2
                    pa, pba = next_bank()
                    for kk in range(8):
                        k.mm(pa, w13s[i][:, kk, 0, i2 * 128:(i2 + 1) * 128], hT[:, kk, :], kk == 0, kk == 7,
                             reads=[b_w13s[i], b_hT[kk]], writes=[pba])
                    pb_, pbb = next_bank()
                    for kk in range(8):
                        k.mm(pb_, w13s[i][:, kk, 1, i2 * 128:(i2 + 1) * 128], hT[:, kk, :], kk == 0, kk == 7,
                             reads=[b_w13s[i], b_hT[kk]], writes=[pbb])
                    u = st["u"] % 2
                    st["u"] += 1
                    k.act(sa[u], pa, AF.Silu, reads=[pba], writes=[b_sa[u]])
                    k.dve(lambda e, f=f, u=u, pb_=pb_: e.tensor_tensor(g[:, f, :], sa[u], pb_, op=ALU.mult),
                          reads=[b_sa[u], pbb], writes=[b_g[f]])
            for n in range(2):
                i = st["w2"] % 2
                st["w2"] += 1
                k.dma("sp", w2s[i], We["w2"][n].rearrange("p (f c) -> p f c", f=NF),
                      reads=[We["b_w2"][n]], writes=[b_w2s[i]])
                for s in range(NSUB):
                    py, pby = next_bank()
                    for f in range(NF):
                        k.mm(py, g[:, f, s * 128:(s + 1) * 128], w2s[i][:, f, :], f == 0, f == NF - 1,
                             reads=[b_g[f], b_w2s[i]], writes=[pby])
                    ydst = yacc[:, s, n * 512:(n + 1) * 512]
                    if not moe:
                        k.dve(lambda e, ydst=ydst, py=py, n=n: e.tensor_tensor(ydst, py, G2[:, n * 512:(n + 1) * 512],
                                                                               op=ALU.mult),
                              reads=[pby, b_G2], writes=[b_y[s]])
                    elif ei == 0:
                        k.dve(lambda e, ydst=ydst, py=py, s=s, ei=ei: e.tensor_scalar_mul(ydst, py, gates[:, s, ei:ei + 1]),
                              reads=[pby, b_gates[s]], writes=[b_y[s]])
                    else:
                        k.dve(lambda e, ydst=ydst, py=py, s=s, ei=ei: e.scalar_tensor_tensor(
                            ydst, in0=py, scalar=gates[:, s, ei:ei + 1], in1=ydst, op0=ALU.mult, op1=ALU.add),
                            reads=[pby, b_gates[s], b_y[s]], writes=[b_y[s]])
        for s in range(NSUB):
            z = s % 2
            if moe:
                k.pool(lambda e, s=s: e.tensor_tensor(yacc[:, s, :], yacc[:, s, :], G2, op=ALU.mult),
                       reads=[b_y[s], b_G2], writes=[b_y[s]])
            o = ln_epilogue(k, None, None, yacc[:, s, :], b_y[s], xt[:, s, :], b_xt, None, None, lng, b_lng, lnb, b_lnb,
                            sm[z], b_sm[z], x_dst[t0 + s * 128:t0 + (s + 1) * 128, :])
    P.barrier()


NCORES = 8
NT_CORE = 4096
DEPTH = 4
FUSED = True
_PROGS = {}


def _prm_inputs(k, sub):
    p = {"ln_g": k.din("ln_g", [1, D]), "ln_b": k.din("ln_b", [1, D])}
    if sub == 0:
        p.update({"b_gates": k.din("b_gates", [1, 8]), "conv_qk": k.din("conv_qk", [4, 2 * D]),
                  "hn_gain": k.din("hn_gain", [1, D]), "conv_short": k.din("conv_short", [3, D])})
    return p


def build_p0():
    k = K(NT_CORE)
    c_d = k.din("c", [1, D])
    w_ada = k.din("w_ada", [DEPTH, D, 6 * D])
    b_ada = k.din("b_ada", [DEPTH, 6 * D])
    modd = k.dout("modd", [DEPTH, 6 * D])
    k.P.barrier()
    phase_p0(k, c_d, w_ada, b_ada, modd, DEPTH)
    return k.finish()


def build_mix():
    k = K(NT_CORE)
    x_d = k.din("x", [NT_CORE, D])
    modd = k.din("modd", [1, 6 * D])
    w_in = k.din("w_in", [D, PW])
    w_out = k.din("w_out", [D, D])
    prm = _prm_inputs(k, 0)
    xfer_in = k.din("xfer_in", [128, XF])
    flag_d = k.din("flag", [128, 1])
    x_o = k.dout("x_out", [NT_CORE, D])
    xfer_out = k.dout("xfer_out", [128, XF])
    k.load_flag(flag_d)
    k.P.barrier()
    W = conv_mixer_weights(k, "m", w_in, w_out)
    k.flush()
    phase_mixer(k, x_d, x_o, modd, prm, W, xfer_in, xfer_out)
    return k.finish()


def build_ffn(moe):
    k = K(NT_CORE)
    x_d = k.din("x", [NT_CORE, D])
    modd = k.din("modd", [1, 6 * D])
    prm = _prm_inputs(k, 1)
    x_o = k.dout("x_out", [NT_CORE, D])
    k.P.barrier()
    if moe:
        w13 = k.din("w13", [NE, D, 2 * DFF])
        w2 = k.din("w2", [NE, DFF, D])
        wr = k.din("wr", [D, NE])
        Ws = [conv_ffn_weights(k, f"e{e}", w13[e], w2[e]) for e in range(NE)]
        k.flush()
        phase_ffn(k, x_d, x_o, modd, prm, Ws, wr)
    else:
        w13 = k.din("w13", [D, 2 * DFF])
        w2 = k.din("w2", [DFF, D])
        Ws = [conv_ffn_weights(k, "d", w13, w2)]
        k.flush()
        phase_ffn(k, x_d, x_o, modd, prm, Ws, None)
    return k.finish()


def _prog(name):
    if name not in _PROGS:
        _PROGS[name] = {"p0": build_p0, "mix": build_mix, "ffn_d": lambda: build_ffn(False),
                        "ffn_m": lambda: build_ffn(True)}[name]()
    return _PROGS[name]


def _run(nc, in_maps):
    res = run_bass_kernel_spmd(nc, in_maps, core_ids=list(range(NCORES)))
    return res.results


def kernel_unfused(inp):
    f32 = lambda a: np.ascontiguousarray(np.asarray(a, dtype=np.float32))
    x = f32(inp["x"])
    B, S, _ = x.shape
    half = S // 2
    xs = [x[c // 2, (c % 2) * half:(c % 2 + 1) * half] for c in range(NCORES)]
    cc = f32(inp["c"])
    w_ada, b_ada = f32(inp["w_ada"]), f32(inp["b_ada"])
    r = _run(_prog("p0"), [{"c": cc[c // 2:c // 2 + 1], "w_ada": w_ada, "b_ada": b_ada} for c in range(NCORES)])
    modd = [r[c]["modd"] for c in range(NCORES)]
    zer = np.zeros((128, XF), np.float32)
    for l in range(DEPTH):
        base = {"w_in": f32(inp["w_in"][l]), "w_out": f32(inp["w_out"][l]), "b_gates": f32(inp["b_gates"][l])[None],
                "conv_qk": f32(inp["conv_qk"][l]), "hn_gain": f32(inp["hn_gain"][l])[None],
                "conv_short": f32(inp["conv_short"][l]), "ln_g": f32(inp["ln_g"][l, 0])[None],
                "ln_b": f32(inp["ln_b"][l, 0])[None]}
        xfer = [zer] * NCORES
        for p in range(2):
            maps = [dict(base, x=xs[c], modd=modd[c][l:l + 1], xfer_in=xfer[c],
                         flag=np.full((128, 1), float(c % 2) if p == 1 else 0.0, np.float32)) for c in range(NCORES)]
            r = _run(_prog("mix"), maps)
            xfer = [r[c & ~1]["xfer_out"] for c in range(NCORES)]
        xm = [r[c]["x_out"] for c in range(NCORES)]
        fb = {"ln_g": f32(inp["ln_g"][l, 1])[None], "ln_b": f32(inp["ln_b"][l, 1])[None]}
        if l % 2 == 0:
            fb.update(w13=f32(inp["dense_w13"][l // 2]), w2=f32(inp["dense_w2"][l // 2]))
            name = "ffn_d"
        else:
            fb.update(w13=f32(inp["moe_w13"][l // 2]), w2=f32(inp["moe_w2"][l // 2]), wr=f32(inp["w_router"][l // 2]))
            name = "ffn_m"
        r = _run(_prog(name), [dict(fb, x=xm[c], modd=modd[c][l:l + 1]) for c in range(NCORES)])
        xs = [r[c]["x_out"] for c in range(NCORES)]
    out = np.empty((B, S, D), np.float32)
    for c in range(NCORES):
        out[c // 2, (c % 2) * half:(c % 2 + 1) * half] = xs[c]
    return out


def build_fused():
    k = K(NT_CORE)
    x_d = k.din("x", [NT_CORE, D])
    c_d = k.din("c", [1, D])
    flag_d = k.din("flag", [128, 1])
    w_ada = k.din("w_ada", [DEPTH, D, 6 * D])
    b_ada = k.din("b_ada", [DEPTH, 6 * D])
    w_in = k.din("w_in", [DEPTH, D, PW])
    b_gates = k.din("b_gates", [DEPTH, 8])
    conv_qk = k.din("conv_qk", [DEPTH, 4, 2 * D])
    hn_gain = k.din("hn_gain", [DEPTH, D])
    conv_short = k.din("conv_short", [DEPTH, 3, D])
    w_out = k.din("w_out", [DEPTH, D, D])
    ln_g = k.din("ln_g", [DEPTH, 2, D])
    ln_b = k.din("ln_b", [DEPTH, 2, D])
    dense_w13 = k.din("dense_w13", [DEPTH // 2, D, 2 * DFF])
    dense_w2 = k.din("dense_w2", [DEPTH // 2, DFF, D])
    w_router = k.din("w_router", [DEPTH // 2, D, NE])
    moe_w13 = k.din("moe_w13", [DEPTH // 2, NE, D, 2 * DFF])
    moe_w2 = k.din("moe_w2", [DEPTH // 2, NE, DFF, D])
    out_d = k.dout("out", [NT_CORE, D])
    modd = k.dint("modd", [DEPTH, 6 * D])
    xm = k.dint("xm", [NT_CORE, D])
    xa = k.dint("xa", [NT_CORE, D])
    send = k.dint("xsend", [128, XF])
    gath = k.dint("xgath", [256, XF])
    b_send, b_gath = Buf("send"), Buf("gath")
    k.load_flag(flag_d)
    k.P.barrier()
    phase_p0(k, c_d, w_ada, b_ada, modd, DEPTH)
    Wm = conv_mixer_weights(k, "m0", w_in[0], w_out[0])
    k.flush()
    x_cur = x_d
    for l in range(DEPTH):
        prm0 = {"b_gates": b_gates[l:l + 1, :], "conv_qk": conv_qk[l], "hn_gain": hn_gain[l:l + 1, :],
                "conv_short": conv_short[l], "ln_g": ln_g[l, 0:1, :], "ln_b": ln_b[l, 0:1, :]}
        prm1 = {"ln_g": ln_g[l, 1:2, :], "ln_b": ln_b[l, 1:2, :]}
        if l % 2 == 0:
            Wf = [conv_ffn_weights(k, f"d{l}", dense_w13[l // 2], dense_w2[l // 2])]
            router = None
        else:
            Wf = [conv_ffn_weights(k, f"e{l}_{e}", moe_w13[l // 2, e], moe_w2[l // 2, e]) for e in range(NE)]
            router = w_router[l // 2]
        k.pump_rate = -(-len(k.deferred) // (2 * k.NTILE))
        phase_mixer(k, x_cur, xm, modd[l:l + 1, :], prm0, Wm, None, send, b_out=b_send, prepass=True)
        k.P.op("cc", lambda e: e.collective_compute("AllGather", ALU.bypass,
                                                   replica_groups=[[0, 1], [2, 3], [4, 5], [6, 7]],
                                                   ins=[send.opt()], outs=[gath.opt()]),
               reads=[b_send], writes=[b_gath])
        phase_mixer(k, x_cur, xm, modd[l:l + 1, :], prm0, Wm, gath[0:128, :], None, b_in=b_gath)
        k.flush()
        if l + 1 < DEPTH:
            Wm = conv_mixer_weights(k, f"m{l + 1}", w_in[l + 1], w_out[l + 1])
            k.pump_rate = -(-len(k.deferred) // k.NTILE)
        dst = out_d if l == DEPTH - 1 else xa
        phase_ffn(k, xm, dst, modd[l:l + 1, :], prm1, Wf, router)
        k.flush()
        x_cur = xa
    return k.finish()


def kernel_fused(inp):
    f32 = lambda a: np.ascontiguousarray(np.asarray(a, dtype=np.float32))
    x = f32(inp["x"])
    B, S, _ = x.shape
    half = S // 2
    if "fused" not in _PROGS:
        _PROGS["fused"] = build_fused()
    shared = {n: f32(inp[n]) for n in ("w_ada", "b_ada", "w_in", "b_gates", "conv_qk", "hn_gain", "conv_short", "w_out",
                                       "ln_g", "ln_b", "dense_w13", "dense_w2", "w_router", "moe_w13", "moe_w2")}
    cc = f32(inp["c"])
    maps = []
    for c in range(NCORES):
        m = dict(shared)
        m["x"] = x[c // 2, (c % 2) * half:(c % 2 + 1) * half]
        m["c"] = cc[c // 2:c // 2 + 1]
        m["flag"] = np.full((128, 1), float(c % 2), np.float32)
        maps.append(m)
    r = _run(_PROGS["fused"], maps)
    out = np.empty((B, S, D), np.float32)
    for c in range(NCORES):
        out[c // 2, (c % 2) * half:(c % 2 + 1) * half] = r[c]["out"]
    return out


def kernel(**inputs):
    if FUSED:
        return kernel_fused(inputs)
    return kernel_unfused(inputs)
```

```python
import contextlib
import numpy as np
import concourse.bass as bass
import concourse.mybir as mybir
from concourse.bass_utils import run_bass_kernel_spmd

F32 = mybir.dt.float32
BF16 = mybir.dt.bfloat16
U8 = mybir.dt.uint8
AF = mybir.ActivationFunctionType
ALU = mybir.AluOpType

D = 1024
H = 4
DH = 256
PW = 9224
DFF = 3584
NE = 8
NF = DFF // 128
LN_EPS = 1e-5
T = 512
NSUB = 4

import os
_DBG = os.environ.get("FFN_DBG", "")
COMPUTE = ("pe", "act", "dve", "pool")
QUEUES = ("sp", "gq", "cc")
QINC = {"sp": 16, "gq": 16, "cc": 1}


class Buf:
    __slots__ = ("name", "writer", "readers")

    def __init__(self, name):
        self.name = name
        self.writer = None
        self.readers = []


class Op:
    __slots__ = ("eng", "fn", "deps", "is_dma", "signal", "count", "sem", "stream")

    def __init__(self, eng, fn, is_dma, stream):
        self.eng = eng
        self.fn = fn
        self.deps = []
        self.is_dma = is_dma
        self.signal = False
        self.count = 0
        self.sem = None
        self.stream = stream


class Prog:
    def __init__(self, nc, n_dma_sems=12):
        self.nc = nc
        self.ops = {e: [] for e in COMPUTE + ("sp",)}
        self.n_dma_sems = n_dma_sems
        self.all_ops = []
        self.bar = []
        self.need_bar = set()
        self.dmas_since_bar = []

    def op(self, eng, fn, reads=(), writes=()):
        is_dma = eng in QUEUES
        stream = "pool" if eng in ("gq", "cc") else eng
        o = Op(eng, fn, is_dma, stream)
        deps = []
        pe = stream == "pe"
        for b in reads:
            w = b.writer
            if w is not None and (w.is_dma or w.stream != stream or not pe):
                deps.append(w)
        for b in writes:
            w = b.writer
            if w is not None and (w.is_dma or w.stream != stream or not pe):
                deps.append(w)
            for r in b.readers:
                if r.is_dma or r.stream != stream or not pe:
                    deps.append(r)
        if stream in self.need_bar:
            deps.extend(self.bar)
            self.need_bar.discard(stream)
        o.deps = deps
        for b in reads:
            b.readers.append(o)
        for b in writes:
            b.writer = o
            b.readers = []
        self.all_ops.append(o)
        self.ops[stream].append(o)
        if is_dma:
            self.dmas_since_bar.append(o)
        return o

    def barrier(self):
        bar = list(self.dmas_since_bar)
        for s, lst in self.ops.items():
            for o in reversed(lst):
                if not o.is_dma:
                    bar.append(o)
                    break
        self.bar = bar
        self.need_bar = set(self.ops.keys())
        self.dmas_since_bar = []

    def emit(self, final_wait_ops=()):
        nc = self.nc
        streams = list(self.ops.keys())
        for o in self.all_ops:
            for d in o.deps:
                d.signal = True
        for o in final_wait_ops:
            o.signal = True
        with contextlib.ExitStack() as es:
            esem = {s: es.enter_context(nc.semaphore("s_" + s)) for s in streams}
            dsem = {q: [es.enter_context(nc.semaphore(f"d_{q}{i}")) for i in range(self.n_dma_sems)]
                    for q in QUEUES}
            ecount = {s: 0 for s in streams}
            dcount = {q: [0] * self.n_dma_sems for q in QUEUES}
            drr = {q: 0 for q in QUEUES}
            dprev = {q: [None] * self.n_dma_sems for q in QUEUES}
            for o in self.all_ops:
                if o.is_dma:
                    q = o.eng
                    i = drr[q]
                    drr[q] = (i + 1) % self.n_dma_sems
                    prev = dprev[q][i]
                    if prev is not None:
                        o.deps.append(prev)
                    dcount[q][i] += QINC[q]
                    o.sem = dsem[q][i]
                    o.count = dcount[q][i]
                    dprev[q][i] = o
                    o.signal = True
                elif o.signal:
                    ecount[o.stream] += 1
                    o.sem = esem[o.stream]
                    o.count = ecount[o.stream]
            self.n_waits = 0
            blk = es.enter_context(nc.Block())

            def run_stream(stream):
                def body(e):
                    waited = {}
                    for o in self.ops[stream]:
                        need = {}
                        for d in o.deps:
                            k = d.sem.name
                            if need.get(k, (None, 0))[1] < d.count:
                                need[k] = (d.sem, d.count)
                        for k, (sem, cnt) in need.items():
                            if waited.get(k, 0) < cnt:
                                e.wait_ge(sem, cnt)
                                waited[k] = cnt
                                self.n_waits += 1
                        ins = o.fn(e)
                        if o.signal:
                            ins.then_inc(o.sem, QINC[o.eng] if o.is_dma else 1)
                    if stream == "sp":
                        for o in final_wait_ops:
                            if waited.get(o.sem.name, 0) < o.count:
                                e.wait_ge(o.sem, o.count)
                                waited[o.sem.name] = o.count
                return body

            blk.tensor(run_stream("pe"))
            blk.scalar(run_stream("act"))
            blk.vector(run_stream("dve"))
            blk.gpsimd(run_stream("pool"))
            blk.sync(run_stream("sp"))


def _dtsize(dt):
    return {F32: 4, BF16: 2, U8: 1}[dt]


class Arena:
    def __init__(self, ap_u8, nbytes):
        self.base = ap_u8
        self.nbytes = nbytes
        self.off = 0

    def reset(self):
        self.off = 0

    def t(self, shape, dt):
        n = 1
        for s in shape[1:]:
            n *= s
        nb = n * _dtsize(dt)
        off = (self.off + 63) // 64 * 64
        assert off + nb <= self.nbytes, f"arena overflow: need {off + nb} > {self.nbytes}"
        self.off = off + nb
        v = self.base[:, off:off + nb].bitcast(dt)
        if len(shape) == 3:
            v = v.rearrange("p (a b) -> p a b", a=shape[1])
        elif len(shape) == 4:
            v = v.rearrange("p (a b c) -> p a b c", a=shape[1], b=shape[2])
        if shape[0] != 128:
            v = v[0:shape[0]]
        return v


class K:
    def __init__(self, NT):
        self.NT = NT
        self.NTILE = NT // T
        self.nc = bass.Bass("TRN2", target_bir_lowering=False)
        self.P = Prog(self.nc)
        self.es = contextlib.ExitStack()
        self.outs = []
        self.deferred = []
        self.pump_rate = 0
        nc = self.nc
        es = self.es
        self.ident = es.enter_context(nc.sbuf_tensor("ident", [128, 128], F32))
        self.identb = es.enter_context(nc.sbuf_tensor("identb", [128, 128], BF16))
        self.triu = es.enter_context(nc.sbuf_tensor("triu", [128, 128], F32))
        self.ones = es.enter_context(nc.sbuf_tensor("ones", [128, 128], F32))
        self.onesb = es.enter_context(nc.sbuf_tensor("onesb", [128, 2], BF16))
        self.epsc = es.enter_context(nc.sbuf_tensor("epsc", [128, 1], F32))
        self.flag = es.enter_context(nc.sbuf_tensor("flag_sb", [128, 1], F32))
        self.nl16 = es.enter_context(nc.sbuf_tensor("nl16", [128, 1], F32))
        self.b_const = Buf("const")
        self.b_flag = Buf("flag")
        self.b_modd = Buf("modd")
        self.b_xfer = Buf("xfer")
        ARENA = 196 * 1024
        self.arena_t = es.enter_context(nc.sbuf_tensor("arena", [128, ARENA], U8))
        self.A = Arena(self.arena_t, ARENA)
        self.psum = [es.enter_context(nc.psum_tensor(f"ps{i}", [128, 1024], F32)) for i in range(4)]
        self.pbuf = [[Buf(f"ps{i}_{h}") for h in range(2)] for i in range(4)]
        self.b_PA1 = [self.pbuf[0][1], self.pbuf[0][1]]
        P = self.P
        cb = [self.b_const]
        P.op("pool", lambda e: e.memset(self.ident[:], 0.0), writes=cb)
        P.op("pool", lambda e: e.affine_select(out=self.ident[:], in_=self.ident[:], pattern=[[-1, 128]],
                                               compare_op=ALU.not_equal, fill=1.0, base=0, channel_multiplier=1),
             reads=cb, writes=cb)
        P.op("pool", lambda e: e.tensor_copy(self.identb[:], self.ident[:]), reads=cb, writes=cb)
        P.op("pool", lambda e: e.memset(self.ones[:], 1.0), writes=cb)
        P.op("pool", lambda e: e.affine_select(out=self.triu[:], in_=self.ones[:], pattern=[[1, 128]],
                                               compare_op=ALU.is_ge, fill=0.0, base=0, channel_multiplier=-1),
             reads=cb, writes=cb)
        P.op("pool", lambda e: e.memset(self.onesb[:], 1.0), writes=cb)
        P.op("pool", lambda e: e.memset(self.epsc[:], LN_EPS), writes=cb)
        P.op("pool", lambda e: e.memset(self.nl16[:], -float(np.log(16.0))), writes=cb)

    def din(self, name, shape, dt=F32):
        return self.nc.dram_tensor(name, list(shape), dt, kind="ExternalInput").ap()

    def dout(self, name, shape, dt=F32):
        return self.nc.dram_tensor(name, list(shape), dt, kind="ExternalOutput").ap()

    def dint(self, name, shape, dt=F32):
        return self.nc.dram_tensor(name, list(shape), dt, kind="Internal").ap()

    def mm(self, out, lhsT, rhs, start, stop, reads, writes):
        return self.P.op("pe", lambda e: e.matmul(out, lhsT=lhsT, rhs=rhs, start=start, stop=stop),
                         reads=reads, writes=writes)

    def tr(self, out, in_, ident, reads, writes):
        return self.P.op("pe", lambda e: e.transpose(out, in_, ident), reads=reads, writes=writes)

    def act(self, out, in_, func, reads, writes, bias=None, scale=None):
        kw = {}
        if bias is not None:
            kw["bias"] = bias
        if scale is not None:
            kw["scale"] = scale
        return self.P.op("act", lambda e: e.activation(out, in_, func, **kw), reads=reads, writes=writes)

    def dve(self, fn, reads, writes):
        return self.P.op("dve", fn, reads=reads, writes=writes)

    def pool(self, fn, reads, writes):
        return self.P.op("pool", fn, reads=reads, writes=writes)

    def dma(self, q, out, in_, reads=(), writes=()):
        return self.P.op(q, lambda e: e.dma_start(out=out, in_=in_), reads=reads, writes=writes)

    def defer(self, q, out, in_, writes):
        self.deferred.append((q, out, in_, writes))

    def pump(self, n):
        for _ in range(min(n, len(self.deferred))):
            q, out, in_, writes = self.deferred.pop(0)
            self.dma(q, out, in_, writes=writes)

    def flush(self):
        self.pump(len(self.deferred))

    def load_flag(self, flag_d):
        self.dma("sp", self.flag[:], flag_d, writes=[self.b_flag])

    def finish(self):
        self.P.emit(final_wait_ops=self.outs)
        self.es.close()
        return self.nc

    def rows_to_cols(self, rows, R, cols, b_rows, b_cols, pbank, pb):
        for c in range(8):
            self.tr(pbank[:, c * R:(c + 1) * R], rows[0:R, c * 128:(c + 1) * 128], self.ident[0:R, 0:R],
                    reads=[b_rows, self.b_const], writes=[pb])
        self.dve(lambda e: e.tensor_copy(cols, pbank[:, 0:8 * R].rearrange("p (c r) -> p c r", c=8)),
                 reads=[pb], writes=[b_cols])


def phase_p0(k, c_d, w_ada_d, b_ada_d, modd, nl):
    A = k.A
    A.reset()
    crow = A.t([8, 128], F32)
    condT = A.t([128, 8], F32)
    wa = [A.t([128, 8, 512], F32) for _ in range(2)]
    brow = A.t([1, 6 * D], F32)
    mrow = A.t([1, 6 * D], F32)
    b_crow, b_cond, b_brow, b_mrow = Buf("crow"), Buf("condT"), Buf("brow"), Buf("mrow")
    b_wa = [Buf("wa0"), Buf("wa1")]
    ps = k.psum[0]
    pb = k.pbuf[0]
    k.dma("sp", crow, c_d[0].rearrange("(k p) -> k p", p=128), writes=[b_crow])
    k.tr(ps[:, 0:8], crow[0:8, 0:128], k.ident[0:8, 0:8], reads=[b_crow, k.b_const], writes=[pb[0]])
    k.act(condT, ps[:, 0:8], AF.Silu, reads=[pb[0]], writes=[b_cond])
    n = 0
    for l in range(nl):
        k.dma("sp", brow, b_ada_d[l:l + 1, :], writes=[b_brow])
        for j in range(12):
            s = n % 2
            n += 1
            k.dma("sp", wa[s], w_ada_d[l][:, j * 512:(j + 1) * 512].rearrange("(k p) n -> p k n", p=128),
                  writes=[b_wa[s]])
            pbank = ps[0:1, s * 512:(s + 1) * 512]
            for kk in range(8):
                k.mm(pbank, condT[:, kk:kk + 1], wa[s][:, kk, :], kk == 0, kk == 7,
                     reads=[b_cond, b_wa[s]], writes=[pb[s]])
            k.dve(lambda e, j=j, pbank=pbank: e.tensor_tensor(mrow[0:1, j * 512:(j + 1) * 512], pbank,
                                                              brow[0:1, j * 512:(j + 1) * 512], op=ALU.add),
                  reads=[pb[s], b_brow], writes=[b_mrow])
        o = k.dma("sp", modd[l:l + 1, :], mrow, reads=[b_mrow], writes=[k.b_modd])
        k.outs.append(o)
    k.P.barrier()


XF = 2048 + 8 + 48 + 16
ALPHA = 8.0 ** 0.25


def conv_mixer_weights(k, tag, w_in_l, w_out_l):
    W = {}
    W["win"] = k.dint(f"winb_{tag}", [20, 128, 4096], BF16)
    W["wg"] = k.dint(f"wgb_{tag}", [128, 64], BF16)
    W["wout"] = k.dint(f"woutb_{tag}", [128, 8192], BF16)
    W["b_win"] = [Buf(f"winb{i}") for i in range(20)]
    W["b_wg"] = Buf("wgb")
    W["b_wout"] = Buf("woutb")

    def srcv(c0, n):
        return w_in_l[:, c0:c0 + n].rearrange("(k p) n -> p k n", p=128)

    def dstv(blk):
        return W["win"][blk].rearrange("p (k n) -> p k n", k=8)

    for blk in range(8):
        k.defer("gq", dstv(blk), srcv(blk * 512, 512), writes=[W["b_win"][blk]])
    k.defer("gq", W["wg"].rearrange("p (k n) -> p k n", k=8), srcv(4096, 8), writes=[W["b_wg"]])
    for j in range(8):
        for i, base in enumerate((4104, 5128, 6152)):
            k.defer("gq", dstv(8 + j)[:, :, i * 128:(i + 1) * 128], srcv(base + j * 128, 128),
                  writes=[W["b_win"][8 + j]])
    for j in range(4):
        k.defer("gq", dstv(16 + j), srcv(7176 + j * 512, 512), writes=[W["b_win"][16 + j]])
    k.defer("gq", W["wout"].rearrange("p (k n) -> p k n", k=8),
          w_out_l.rearrange("(k p) n -> p k n", p=128), writes=[W["b_wout"]])
    return W


def phase_mixer(k, x_src, x_dst, mod_l, prm, W, xfer_in, xfer_out, b_in=None, b_out=None, prepass=False):
    A = k.A
    A.reset()
    P = k.P
    PA, PB, PC, PD = k.psum
    bPA, bPB, bPC, bPD = k.pbuf
    bPAh = [[bPA[0]], [bPA[1]]]
    xt = A.t([128, NSUB, D], F32)
    b_xt = Buf("xt")
    hT = A.t([128, 8, T], BF16)
    b_hT = [Buf(f"hT{c}") for c in range(8)]
    wsl = [A.t([128, 8, 512], BF16) for _ in range(3)]
    b_wsl = [Buf(f"wsl{i}") for i in range(3)]
    wg = A.t([128, 8, 8], BF16)
    b_wgs = Buf("wg")
    wo = A.t([128, 8, D], BF16)
    b_wo = Buf("wo")
    qT = A.t([128, 8, T], BF16)
    kT = A.t([128, 8, T], BF16)
    g1 = A.t([128, 8, T], BF16)
    m2 = A.t([128, 8, T], BF16)
    b_qT = [Buf(f"qT{c}") for c in range(8)]
    b_kT = [Buf(f"kT{c}") for c in range(8)]
    b_g1 = [Buf(f"g1{c}") for c in range(8)]
    b_m2 = [Buf(f"m2{c}") for c in range(8)]
    v = A.t([128, NSUB, D], BF16)
    b_v = [Buf(f"v{s}") for s in range(NSUB)]
    U = [A.t([128, T + 3], F32) for _ in range(2)]
    b_U = [Buf("U0"), Buf("U1")]
    acc = [A.t([128, T], F32) for _ in range(2)]
    b_acc = [Buf("acc0"), Buf("acc1")]
    ccs = [A.t([128, T], F32) for _ in range(2)]
    b_ccs = [Buf("ccs0"), Buf("ccs1")]
    sgt = [A.t([128, T], BF16) for _ in range(2)]
    b_sgt = [Buf("sgt0"), Buf("sgt1")]
    hnTok = A.t([128, NSUB, D], BF16)
    b_hn = [Buf(f"hn{s}") for s in range(NSUB)]
    mT = A.t([128, 8, T], BF16)
    b_mT = [Buf(f"mT{c}") for c in range(8)]
    ya = [A.t([128, T], BF16) for _ in range(2)]
    b_ya = [Buf("ya0"), Buf("ya1")]
    xf = A.t([128, XF], F32)
    Cst = xf[:, 0:2048].rearrange("p (j h d) -> p j h d", j=2, h=4)
    nS = xf[:, 2048:2056].rearrange("p (j h) -> p j h", j=2)
    haloU = xf[:, 2056:2104].rearrange("p (c t) -> p c t", c=16)
    haloU2 = xf[:, 2104:2120].rearrange("p (c t) -> p c t", c=8)
    b_C = [Buf(f"C{h}") for h in range(4)]
    b_nS = Buf("nS")
    b_hU = [Buf(f"hU{c}") for c in range(16)]
    b_hU2 = [Buf(f"hU2{c}") for c in range(8)]
    Cb = A.t([128, 2, 4, DH], BF16)
    b_Cb = [Buf(f"Cb{h}") for h in range(4)]
    nb = A.t([128, 2, 4], BF16)
    b_nb = Buf("nb")
    kS = [A.t([128, 4, DH], BF16) for _ in range(2)]
    b_kS = [Buf("kS0"), Buf("kS1")]
    Pm = [A.t([128, 4, 128], BF16) for _ in range(2)]
    b_Pm = [Buf("Pm0"), Buf("Pm1")]
    gates8 = A.t([128, NSUB, 8], F32)
    b_g8 = [Buf(f"g8{s}") for s in range(NSUB)]
    sm = [A.t([128, 96], F32) for _ in range(2)]
    b_sm = [Buf("sm0"), Buf("sm1")]
    rb = [A.t([128, D], F32) for _ in range(2)]
    b_rb = [Buf("rb0"), Buf("rb1")]
    G1 = A.t([128, D], F32)
    lng = A.t([128, D], F32)
    lnb = A.t([128, D], F32)
    bg = A.t([128, 8], F32)
    b_G1, b_lng, b_lnb, b_bg = Buf("G1"), Buf("lng"), Buf("lnb"), Buf("bg")
    rows = A.t([14, D], F32)
    b_rows = Buf("rows")
    cols = A.t([128, 8, 14], F32)
    b_cols = Buf("cols")
    sc1 = A.t([128, 8], F32)
    b_sc1 = Buf("sc1")
    cb = [k.b_const]

    k.dma("sp", rows[0:1, :], mod_l[:, 0:D], reads=[k.b_modd], writes=[b_rows])
    k.dma("sp", rows[1:2, :], mod_l[:, D:2 * D], reads=[k.b_modd], writes=[b_rows])
    k.dma("sp", rows[2:3, :], prm["hn_gain"], writes=[b_rows])
    k.dma("sp", rows[3:6, :], prm["conv_short"], writes=[b_rows])
    k.dma("sp", rows[6:10, :], prm["conv_qk"][:, 0:D], writes=[b_rows])
    k.dma("sp", rows[10:14, :], prm["conv_qk"][:, D:2 * D], writes=[b_rows])
    k.rows_to_cols(rows, 14, cols, b_rows, b_cols, PA[:, 0:512], bPA[0])
    k.dve(lambda e: e.tensor_scalar_add(sc1, cols[:, :, 1], 1.0), reads=[b_cols], writes=[b_sc1])
    k.dma("sp", G1, mod_l[:, 2 * D:3 * D].partition_broadcast(128), reads=[k.b_modd], writes=[b_G1])
    k.pool(lambda e: e.tensor_scalar_add(G1, G1, 1.0), reads=[b_G1], writes=[b_G1])
    k.dma("sp", lng, prm["ln_g"].partition_broadcast(128), writes=[b_lng])
    k.dma("sp", lnb, prm["ln_b"].partition_broadcast(128), writes=[b_lnb])
    k.dma("sp", bg, prm["b_gates"].partition_broadcast(128), writes=[b_bg])
    if not prepass:
        k.dma("sp", wo, W["wout"].rearrange("p (k n) -> p k n", k=8), reads=[W["b_wout"]], writes=[b_wo])
    k.dma("sp", wg, W["wg"].rearrange("p (k n) -> p k n", k=8), reads=[W["b_wg"]], writes=[b_wgs])
    b_xf_all = b_C + [b_nS] + b_hU + b_hU2
    if xfer_in is None:
        k.dve(lambda e: e.memset(xf, 0.0), reads=[], writes=b_xf_all)
    else:
        k.dma("sp", xf, xfer_in, reads=[b_in or k.b_xfer], writes=b_xf_all)
        k.dve(lambda e: e.tensor_scalar_mul(xf, xf, k.flag[:, 0:1]), reads=b_xf_all + [k.b_flag], writes=b_xf_all)
    for h in range(4):
        k.act(Cb[:, :, h, :], Cst[:, :, h, :], AF.Copy, reads=[b_C[h]], writes=[b_Cb[h]])
    k.act(nb, nS, AF.Copy, reads=[b_nS], writes=[b_nb])

    banks = [(PB[:, 0:512], bPB[0]), (PB[:, 512:1024], bPB[1]), (PC[:, 0:512], bPC[0]), (PC[:, 512:1024], bPC[1])]
    st = {"bank": 0, "w": 0, "u": 0}

    def next_bank():
        b = banks[st["bank"] % 4]
        st["bank"] += 1
        return b

    def load_w(blk, ncols=512):
        i = st["w"] % 3
        st["w"] += 1
        k.dma("sp", wsl[i][:, :, 0:ncols], W["win"][blk].rearrange("p (k n) -> p k n", k=8)[:, :, 0:ncols],
              reads=[W["b_win"][blk]], writes=[b_wsl[i]])
        return wsl[i], b_wsl[i]

    def proj_fm(wt, bw, c0):
        pbank, pb = next_bank()
        for kk in range(8):
            k.mm(pbank, wt[:, kk, c0:c0 + 128], hT[:, kk, :], kk == 0, kk == 7,
                 reads=[bw, b_hT[kk]], writes=[pb])
        return pbank, pb

    LN16 = float(np.log(16.0))
    pden = PD[:, 16:20]
    pbp = PD[:, 8:16]
    pdn = PD[:, 20:28].rearrange("p (j h) -> p j h", j=2)
    b_pg = [bPD[0]] * NSUB
    b_pbp = b_pden = b_pdn = bPD[0]
    PD1b = PD[:, 512:1024].bitcast(BF16)
    PA1b = PA[:, 512:1024].bitcast(BF16)
    b_PA1b = k.b_PA1

    for ti in range(k.NTILE):
        t0 = ti * T
        k.pump(k.pump_rate)
        k.dma("sp", xt, x_src[t0:t0 + T, :].rearrange("(s p) d -> p s d", p=128), writes=[b_xt])
        for c in range(8):
            hb = c % 2
            pbank = PA[:, hb * 512:(hb + 1) * 512]
            for s in range(NSUB):
                k.tr(pbank[:, s * 128:(s + 1) * 128], xt[:, s, c * 128:(c + 1) * 128], k.ident[:],
                     reads=[b_xt, k.b_const], writes=bPAh[hb])
            k.act(hT[:, c, :], pbank, AF.Identity, reads=bPAh[hb] + [b_sc1, b_cols], writes=[b_hT[c]],
                  scale=sc1[:, c:c + 1], bias=cols[:, c, 0:1])
        last = ti == k.NTILE - 1
        for blk in range(4):
            if prepass and blk < 2:
                if last:
                    wt, bw = load_w(blk)
                    for cc in range(4):
                        c16 = blk * 4 + cc
                        pbank, pb = next_bank()
                        for kk in range(8):
                            k.mm(pbank[:, 0:3], wt[:, kk, cc * 128:(cc + 1) * 128], hT[:, kk, T - 3:T], kk == 0, kk == 7,
                                 reads=[bw, b_hT[kk]], writes=[pb])
                        k.dve(lambda e, c16=c16, pbank=pbank: e.tensor_copy(haloU[:, c16, :], pbank[:, 0:3]),
                              reads=[pb], writes=[b_hU[c16]])
                continue
            wt, bw = load_w(blk)
            for cc in range(4):
                c16 = blk * 4 + cc
                c8 = c16 % 8
                rbase = 6 if c16 < 8 else 10
                dstT, b_dst = (qT, b_qT) if c16 < 8 else (kT, b_kT)
                pbank, pb = proj_fm(wt, bw, cc * 128)
                u = st["u"] % 2
                st["u"] += 1
                Ut, bU, ac, bac = U[u], b_U[u], acc[u], b_acc[u]
                k.pool(lambda e, Ut=Ut, c16=c16: e.tensor_copy(Ut[:, 0:3], haloU[:, c16, :]),
                       reads=[b_hU[c16]], writes=[bU])
                k.act(Ut[:, 3:T + 3], pbank, AF.Copy, reads=[pb], writes=[bU])
                k.pool(lambda e, Ut=Ut, c16=c16: e.tensor_copy(haloU[:, c16, :], Ut[:, T:T + 3]),
                       reads=[bU], writes=[b_hU[c16]])
                k.act(ac, pbank, AF.Identity, reads=[pb, b_cols], writes=[bac], scale=cols[:, c8, rbase + 3:rbase + 4])
                for j in range(0, 3):
                    k.dve(lambda e, Ut=Ut, ac=ac, c8=c8, rbase=rbase, j=j: e.scalar_tensor_tensor(
                        ac, in0=Ut[:, j:j + T], scalar=cols[:, c8, rbase + j:rbase + j + 1], in1=ac,
                        op0=ALU.mult, op1=ALU.add), reads=[bU, b_cols, bac], writes=[bac])
                k.act(dstT[:, c8, :], ac, AF.Silu, reads=[bac], writes=[b_dst[c8]])
        for blk in (4, 5):
            wt, bw = load_w(blk)
            for s in range(NSUB):
                pbank, pb = next_bank()
                for kk in range(8):
                    k.mm(pbank, hT[:, kk, s * 128:(s + 1) * 128], wt[:, kk, :], kk == 0, kk == 7,
                         reads=[bw, b_hT[kk]], writes=[pb])
                k.act(v[:, s, (blk - 4) * 512:(blk - 3) * 512], pbank, AF.Copy, reads=[pb], writes=[b_v[s]])
        for blk in (() if prepass else (6, 7)):
            wt, bw = load_w(blk)
            for cc in range(4):
                c8 = (blk - 6) * 4 + cc
                pbank, pb = proj_fm(wt, bw, cc * 128)
                k.act(g1[:, c8, :], pbank, AF.Sigmoid, reads=[pb], writes=[b_g1[c8]])
        for s in range(NSUB):
            pg = PD[:, 32 + s * 8:32 + (s + 1) * 8]
            for kk in range(8):
                k.mm(pg, hT[:, kk, s * 128:(s + 1) * 128], wg[:, kk, :], kk == 0, kk == 7,
                     reads=[b_wgs, b_hT[kk]], writes=[b_pg[s]])
            k.dve(lambda e, s=s, pg=pg: e.tensor_tensor(gates8[:, s, :], pg, bg, op=ALU.add),
                  reads=[b_pg[s], b_bg], writes=[b_g8[s]])
        for j in range(8):
            if prepass:
                if last:
                    wt, bw = load_w(8 + j, 384)
                    pcc, pbcc = next_bank()
                    pcx, pbcx = next_bank()
                    for (pbk, pbb, c0) in ((pcc, pbcc, 128), (pcx, pbcx, 256)):
                        for kk in range(8):
                            k.mm(pbk[:, 0:2], wt[:, kk, c0:c0 + 128], hT[:, kk, T - 2:T], kk == 0, kk == 7,
                                 reads=[bw, b_hT[kk]], writes=[pbb])
                    u = st["u"] % 2
                    st["u"] += 1
                    k.act(ccs[u][:, 0:2], pcc[:, 0:2], AF.Copy, reads=[pbcc], writes=[b_ccs[u]])
                    k.dve(lambda e, j=j, u=u, pcx=pcx: e.tensor_tensor(haloU2[:, j, :], pcx[:, 0:2], ccs[u][:, 0:2], op=ALU.mult),
                          reads=[pbcx, b_ccs[u]], writes=[b_hU2[j]])
                continue
            wt, bw = load_w(8 + j, 384)
            pcb, pbcb = proj_fm(wt, bw, 0)
            pcc, pbcc = proj_fm(wt, bw, 128)
            pcx, pbcx = proj_fm(wt, bw, 256)
            u = st["u"] % 2
            st["u"] += 1
            Ut, bU, ac, bac, cs, bcs = U[u], b_U[u], acc[u], b_acc[u], ccs[u], b_ccs[u]
            k.act(cs, pcc, AF.Copy, reads=[pbcc], writes=[bcs])
            k.pool(lambda e, Ut=Ut, j=j: e.tensor_copy(Ut[:, 0:2], haloU2[:, j, :]), reads=[b_hU2[j]], writes=[bU])
            k.dve(lambda e, Ut=Ut, cs=cs, pcx=pcx: e.tensor_tensor(Ut[:, 2:T + 2], pcx, cs, op=ALU.mult),
                  reads=[pbcx, bcs], writes=[bU])
            k.pool(lambda e, Ut=Ut, j=j: e.tensor_copy(haloU2[:, j, :], Ut[:, T:T + 2]), reads=[bU], writes=[b_hU2[j]])
            k.act(ac, Ut[:, 2:T + 2], AF.Identity, reads=[bU, b_cols], writes=[bac], scale=cols[:, j, 5:6])
            for tp in (0, 1):
                k.dve(lambda e, Ut=Ut, ac=ac, j=j, tp=tp: e.scalar_tensor_tensor(
                    ac, in0=Ut[:, tp:tp + T], scalar=cols[:, j, 3 + tp:4 + tp], in1=ac, op0=ALU.mult, op1=ALU.add),
                    reads=[bU, b_cols, bac], writes=[bac])
            k.dve(lambda e, ac=ac, pcb=pcb, j=j: e.tensor_tensor(m2[:, j, :], ac, pcb, op=ALU.mult),
                  reads=[bac, pbcb], writes=[b_m2[j]])
        for blk in (() if prepass else range(16, 20)):
            wt, bw = load_w(blk)
            for cc in range(4):
                c8 = ((blk - 16) % 2) * 4 + cc
                tgt, b_tgt = (g1, b_g1) if blk < 18 else (m2, b_m2)
                pbank, pb = proj_fm(wt, bw, cc * 128)
                u = st["u"] % 2
                st["u"] += 1
                k.act(sgt[u], pbank, AF.Sigmoid, reads=[pb], writes=[b_sgt[u]])
                k.pool(lambda e, tgt=tgt, c8=c8, u=u: e.tensor_tensor(tgt[:, c8, :], tgt[:, c8, :], sgt[u], op=ALU.mult),
                       reads=[b_tgt[c8], b_sgt[u]], writes=[b_tgt[c8]])

        for s in range(NSUB):
            sub = slice(s * 128, (s + 1) * 128)
            z = (ti * NSUB + s) % 2
            S = sm[z]
            bS = b_sm[z]
            e1, l1, tmp4, ws_, ebt, ebL = S[:, 0:4], S[:, 4:8], S[:, 8:12], S[:, 12:16], S[:, 16:20], S[:, 20:24]
            d1, d2, scl, t1, t3, aa = S[:, 24:28], S[:, 28:32], S[:, 32:36], S[:, 36:40], S[:, 40:44], S[:, 44:48]
            stt = S[:, 48:72].rearrange("p (h x) -> p h x", h=4)
            mv = S[:, 72:80].rearrange("p (h x) -> p h x", h=4)
            k.act(e1, gates8[:, s, 4:8], AF.Exp, reads=[b_g8[s]], writes=[bS], scale=-1.0)
            k.act(l1, e1, AF.Ln, reads=[bS], writes=[bS], bias=1.0)
            k.mm(pbp[:, 0:4], k.triu[:], l1, True, True, reads=[bS, k.b_const], writes=[b_pbp])
            k.mm(pbp[:, 4:8], k.ones[:], l1, True, True, reads=[bS, k.b_const], writes=[b_pbp])
            k.dve(lambda e, tmp4=tmp4, s=s: e.tensor_tensor(tmp4, gates8[:, s, 0:4], pbp[:, 0:4], op=ALU.add),
                  reads=[b_g8[s], b_pbp], writes=[bS])
            k.act(ws_, tmp4, AF.Exp, reads=[bS], writes=[bS], bias=k.nl16[:, 0:1])
            if not prepass:
                k.act(ebt, pbp[:, 0:4], AF.Exp, reads=[b_pbp], writes=[bS], scale=-1.0)
            k.act(ebL, pbp[:, 4:8], AF.Exp, reads=[b_pbp], writes=[bS], scale=-1.0)
            for h in range(4):
                for j in range(2):
                    k.tr(PD1b[:, (h * 2 + j) * 128:(h * 2 + j + 1) * 128], kT[:, 2 * h + j, sub], k.identb[:],
                         reads=[b_kT[2 * h + j], k.b_const], writes=[bPD[1]])
            for h in range(4):
                k.dve(lambda e, h=h, z=z, ws_=ws_: e.tensor_scalar_mul(kS[z][:, h, :], PD1b[:, h * 256:(h + 1) * 256],
                                                                       ws_[:, h:h + 1]),
                      reads=[bPD[1], bS], writes=[b_kS[z]])
            for h in (() if prepass else range(4)):
                for j in range(2):
                    k.mm(PA[:, h * 128:(h + 1) * 128], kT[:, 2 * h + j, sub], qT[:, 2 * h + j, sub], j == 0, j == 1,
                         reads=[b_kT[2 * h + j], b_qT[2 * h + j]], writes=[bPA[0]])
            for h in (() if prepass else range(4)):
                k.dve(lambda e, h=h, z=z, ws_=ws_: e.scalar_tensor_tensor(
                    Pm[z][:, h, :], in0=PA[:, h * 128:(h + 1) * 128], scalar=ws_[:, h:h + 1], in1=k.triu[:],
                    op0=ALU.mult, op1=ALU.mult), reads=[bPA[0], bS, k.b_const], writes=[b_Pm[z]])
            for h in (() if prepass else range(4)):
                hb = h // 2
                nump = PB[:, h * 256:(h + 1) * 256]
                k.mm(nump, Pm[z][:, h, :], v[:, s, h * 256:(h + 1) * 256], True, False,
                     reads=[b_Pm[z], b_v[s]], writes=[bPB[hb]])
                for j in range(2):
                    k.mm(nump, qT[:, 2 * h + j, sub], Cb[:, j, h, :], False, j == 1,
                         reads=[b_qT[2 * h + j], b_Cb[h]], writes=[bPB[hb]])
                k.mm(pden[:, h:h + 1], Pm[z][:, h, :], k.onesb[:, 0:1], True, False,
                     reads=[b_Pm[z], k.b_const], writes=[b_pden])
                for j in range(2):
                    k.mm(pden[:, h:h + 1], qT[:, 2 * h + j, sub], nb[:, j, h:h + 1], False, j == 1,
                         reads=[b_qT[2 * h + j], b_nb], writes=[b_pden])
            for h in range(4):
                hb = h % 2
                pdel = PC[:, hb * 512:(hb + 1) * 512]
                for j in range(2):
                    k.mm(pdel[:, j * 256:(j + 1) * 256], kS[z][:, h, j * 128:(j + 1) * 128],
                         v[:, s, h * 256:(h + 1) * 256], True, True, reads=[b_kS[z], b_v[s]], writes=[bPC[hb]])
                    k.mm(pdn[:, j, h:h + 1], kS[z][:, h, j * 128:(j + 1) * 128], k.onesb[:, 0:1], True, True,
                         reads=[b_kS[z], k.b_const], writes=[b_pdn])
                k.act(Cst[:, :, h, :], Cst[:, :, h, :], AF.Identity, reads=[b_C[h], bS], writes=[b_C[h]],
                      scale=ebL[:, h:h + 1])
                k.dve(lambda e, h=h, ebL=ebL, pdel=pdel: e.scalar_tensor_tensor(
                    Cst[:, :, h, :], in0=pdel.rearrange("p (j d) -> p j d", j=2), scalar=ebL[:, h:h + 1],
                    in1=Cst[:, :, h, :], op0=ALU.mult, op1=ALU.add), reads=[bPC[hb], bS, b_C[h]], writes=[b_C[h]])
                if not prepass:
                    k.act(Cb[:, :, h, :], Cst[:, :, h, :], AF.Copy, reads=[b_C[h]], writes=[b_Cb[h]])
            k.dve(lambda e: e.tensor_tensor(nS, nS, pdn, op=ALU.add), reads=[b_nS, b_pdn], writes=[b_nS])
            k.dve(lambda e, ebL=ebL: e.tensor_tensor(nS, nS, ebL.unsqueeze(1).to_broadcast([128, 2, 4]), op=ALU.mult),
                  reads=[b_nS, bS], writes=[b_nS])
            if prepass:
                continue
            k.act(nb, nS, AF.Copy, reads=[b_nS], writes=[b_nb])
            k.dve(lambda e, d1=d1, ebt=ebt: e.tensor_tensor(d1, pden, ebt, op=ALU.mult), reads=[b_pden, bS], writes=[bS])
            k.act(d2, d1, AF.Abs, reads=[bS], writes=[bS])
            k.dve(lambda e, d2=d2: e.tensor_scalar_max(d2, d2, 1.0), reads=[bS], writes=[bS])
            k.dve(lambda e, d2=d2: e.reciprocal(d2, d2), reads=[bS], writes=[bS])
            k.dve(lambda e, d2=d2, scl=scl, ebt=ebt: e.tensor_tensor(scl, ebt, d2, op=ALU.mult), reads=[bS], writes=[bS])
            for h in range(4):
                k.dve(lambda e, h=h, stt=stt: e.bn_stats(stt[:, h, :], PB[:, h * 256:(h + 1) * 256]),
                      reads=[bPB[h // 2]], writes=[bS])
                k.dve(lambda e, h=h, stt=stt, mv=mv: e.bn_aggr(mv[:, h, :], stt[:, h, :]), reads=[bS], writes=[bS])
            k.dve(lambda e, t1=t1, scl=scl: e.tensor_tensor(t1, scl, scl, op=ALU.mult), reads=[bS], writes=[bS])
            k.dve(lambda e, t1=t1, mv=mv: e.tensor_tensor(t1, t1, mv[:, :, 1], op=ALU.mult), reads=[bS], writes=[bS])
            k.act(t3, t1, AF.Sqrt, reads=[bS, k.b_const], writes=[bS], bias=k.epsc[:, 0:1])
            k.dve(lambda e, t3=t3: e.reciprocal(t3, t3), reads=[bS], writes=[bS])
            k.dve(lambda e, t3=t3, aa=aa, scl=scl: e.tensor_tensor(aa, t3, scl, op=ALU.mult), reads=[bS], writes=[bS])
            for h in range(4):
                k.dve(lambda e, h=h, s=s, mv=mv, aa=aa: e.tensor_scalar(
                    hnTok[:, s, h * 256:(h + 1) * 256], PB[:, h * 256:(h + 1) * 256], mv[:, h, 0:1], aa[:, h:h + 1],
                    op0=ALU.subtract, op1=ALU.mult), reads=[bPB[h // 2], bS], writes=[b_hn[s]])

        if prepass:
            continue
        for c in range(8):
            hb = c % 2
            for s in range(NSUB):
                k.tr(PA1b[:, hb * 512 + s * 128:hb * 512 + (s + 1) * 128], hnTok[:, s, c * 128:(c + 1) * 128], k.identb[:],
                     reads=[b_hn[s], k.b_const], writes=[b_PA1b[hb]])
            k.dve(lambda e, c=c, hb=hb: e.scalar_tensor_tensor(
                ya[hb], in0=PA1b[:, hb * 512:(hb + 1) * 512], scalar=cols[:, c, 2:3], in1=g1[:, c, :],
                op0=ALU.mult, op1=ALU.mult), reads=[b_PA1b[hb], b_cols, b_g1[c]], writes=[b_ya[hb]])
            k.pool(lambda e, c=c, hb=hb: e.tensor_tensor(mT[:, c, :], ya[hb], m2[:, c, :], op=ALU.add),
                   reads=[b_ya[hb], b_m2[c]], writes=[b_mT[c]])

        for s in range(NSUB):
            sub = slice(s * 128, (s + 1) * 128)
            yb_, byb = (PB, bPB) if s % 2 == 0 else (PC, bPC)
            z = s % 2
            for n in range(2):
                for c in range(8):
                    k.mm(yb_[:, n * 512:(n + 1) * 512], mT[:, c, sub], wo[:, c, n * 512:(n + 1) * 512], c == 0, c == 7,
                         reads=[b_mT[c], b_wo], writes=[byb[n]])
            ln_epilogue(k, yb_[:], byb, rb[z], b_rb[z], xt[:, s, :], b_xt, G1, b_G1, lng, b_lng, lnb, b_lnb,
                        sm[z][:, 80:96], b_sm[z], x_dst[t0 + s * 128:t0 + (s + 1) * 128, :])
    if xfer_out is not None:
        o = k.dma("sp", xfer_out, xf, reads=b_xf_all, writes=[b_out or k.b_xfer])
        k.outs.append(o)
    P.barrier()


def ln_epilogue(k, yps, byps, r, b_r, xres, b_xres, G, b_G, lng, b_lng, lnb, b_lnb, S, bS, dst):
    st6 = S[:, 0:12].rearrange("p (a b) -> p a b", a=2)
    mv = S[:, 12:14]
    sq = S[:, 14:15]
    nmr = S[:, 15:16]
    if yps is not None:
        k.dve(lambda e: e.tensor_tensor(r, yps, G, op=ALU.mult), reads=byps + [b_G], writes=[b_r])
    k.dve(lambda e: e.scalar_tensor_tensor(r, in0=xres, scalar=ALPHA, in1=r, op0=ALU.mult, op1=ALU.add),
          reads=[b_r, b_xres], writes=[b_r])
    for a in range(2):
        k.dve(lambda e, a=a: e.bn_stats(st6[:, a, :], r[:, a * 512:(a + 1) * 512]), reads=[b_r], writes=[bS])
    k.dve(lambda e: e.bn_aggr(mv, S[:, 0:12]), reads=[bS], writes=[bS])
    k.act(sq, mv[:, 1:2], AF.Sqrt, reads=[bS, k.b_const], writes=[bS], bias=k.epsc[:, 0:1])
    k.dve(lambda e: e.reciprocal(sq, sq), reads=[bS], writes=[bS])
    k.dve(lambda e: e.tensor_scalar(nmr, mv[:, 0:1], -1.0, sq, op0=ALU.mult, op1=ALU.mult), reads=[bS], writes=[bS])
    k.act(r, r, AF.Identity, reads=[b_r, bS], writes=[b_r], scale=sq, bias=nmr)
    k.pool(lambda e: e.tensor_tensor(r, r, lng, op=ALU.mult), reads=[b_r, b_lng], writes=[b_r])
    k.pool(lambda e: e.tensor_tensor(r, r, lnb, op=ALU.add), reads=[b_r, b_lnb], writes=[b_r])
    o = k.dma("sp", dst, r, reads=[b_r])
    k.outs.append(o)
    return o


def conv_ffn_weights(k, tag, w13_l, w2_l):
    W = {}
    W["w13"] = k.dint(f"w13b_{tag}", [14, 128, 8 * 2 * 256], BF16)
    W["w2"] = k.dint(f"w2b_{tag}", [2, 128, NF * 512], BF16)
    W["b_w13"] = [Buf(f"w13b{g}") for g in range(14)]
    W["b_w2"] = [Buf("w2b0"), Buf("w2b1")]
    for g in range(14):
        dv = W["w13"][g].rearrange("p (k i n) -> p k i n", k=8, i=2)
        for i in range(2):
            k.defer("gq", dv[:, :, i, :], w13_l[:, i * DFF + g * 256:i * DFF + (g + 1) * 256].rearrange(
                "(k p) n -> p k n", p=128), writes=[W["b_w13"][g]])
    for n in range(2):
        dv = W["w2"][n].rearrange("p (f c) -> p f c", f=NF)
        for q in range(4):
            k.defer("gq", dv[:, q * 7:(q + 1) * 7, :],
                  w2_l[q * 7 * 128:(q + 1) * 7 * 128, n * 512:(n + 1) * 512].rearrange("(f p) c -> p f c", p=128),
                  writes=[W["b_w2"][n]])
    return W


def phase_ffn(k, x_src, x_dst, mod_l, prm, experts, router_d, final=False):
    A = k.A
    A.reset()
    P = k.P
    PA, PB, PC, PD = k.psum
    bPA, bPB, bPC, bPD = k.pbuf
    bPAh = [[bPA[0]], [bPA[1]]]
    moe = router_d is not None
    xt = A.t([128, NSUB, D], F32)
    b_xt = Buf("xt")
    hT = A.t([128, 8, T], BF16)
    b_hT = [Buf(f"hT{c}") for c in range(8)]
    g = A.t([128, NF, T], BF16)
    b_g = [Buf(f"g{f}") for f in range(NF)]
    yacc = A.t([128, NSUB, D], F32)
    b_y = [Buf(f"y{s}") for s in range(NSUB)]
    w13s = [A.t([128, 8, 2, 256], BF16) for _ in range(2)]
    b_w13s = [Buf("w13s0"), Buf("w13s1")]
    w2s = [A.t([128, NF, 512], BF16) for _ in range(2)]
    b_w2s = [Buf("w2s0"), Buf("w2s1")]
    G2 = A.t([128, D], F32)
    lng = A.t([128, D], F32)
    lnb = A.t([128, D], F32)
    b_G2, b_lng, b_lnb = Buf("G2"), Buf("lng"), Buf("lnb")
    sa = [A.t([128, T], F32) for _ in range(2)]
    b_sa = [Buf("sa0"), Buf("sa1")]
    rows = A.t([2, D], F32)
    b_rows = Buf("rows")
    cols = A.t([128, 8, 2], F32)
    b_cols = Buf("cols")
    sc1 = A.t([128, 8], F32)
    b_sc1 = Buf("sc1")
    sm = [A.t([128, 16], F32) for _ in range(2)]
    b_sm = [Buf("sm0"), Buf("sm1")]
    if moe:
        hT32 = A.t([128, 8, T], F32)
        b_hT32 = [Buf(f"hT32{c}") for c in range(8)]
        wr = A.t([128, 8, NE], F32)
        b_wr = Buf("wr")
        gates = A.t([128, NSUB, NE], F32)
        b_gates = [Buf(f"gates{s}") for s in range(NSUB)]
        rt = [A.t([128, 64], F32) for _ in range(2)]
        b_rt = [Buf("rt0"), Buf("rt1")]

    k.dma("sp", rows[0:1, :], mod_l[:, 3 * D:4 * D], reads=[k.b_modd], writes=[b_rows])
    k.dma("sp", rows[1:2, :], mod_l[:, 4 * D:5 * D], reads=[k.b_modd], writes=[b_rows])
    k.rows_to_cols(rows, 2, cols, b_rows, b_cols, PA[:, 0:512], bPA[0])
    k.dve(lambda e: e.tensor_scalar_add(sc1, cols[:, :, 1], 1.0), reads=[b_cols], writes=[b_sc1])
    k.dma("sp", G2, mod_l[:, 5 * D:6 * D].partition_broadcast(128), reads=[k.b_modd], writes=[b_G2])
    k.pool(lambda e: e.tensor_scalar_add(G2, G2, 1.0), reads=[b_G2], writes=[b_G2])
    k.dma("sp", lng, prm["ln_g"].partition_broadcast(128), writes=[b_lng])
    k.dma("sp", lnb, prm["ln_b"].partition_broadcast(128), writes=[b_lnb])
    if moe:
        k.dma("sp", wr, router_d.rearrange("(k p) e -> p k e", p=128), writes=[b_wr])

    banks = [(PB[:, 0:512], bPB[0]), (PB[:, 512:1024], bPB[1]), (PC[:, 0:512], bPC[0]), (PC[:, 512:1024], bPC[1])]
    st = {"bank": 0, "w13": 0, "w2": 0, "u": 0}

    def next_bank():
        b = banks[st["bank"] % 4]
        st["bank"] += 1
        return b

    for ti in range(k.NTILE):
        t0 = ti * T
        k.pump(k.pump_rate)
        k.dma("sp", xt, x_src[t0:t0 + T, :].rearrange("(s p) d -> p s d", p=128), writes=[b_xt])
        for c in range(8):
            hb = c % 2
            pbank = PA[:, hb * 512:(hb + 1) * 512]
            for s in range(NSUB):
                k.tr(pbank[:, s * 128:(s + 1) * 128], xt[:, s, c * 128:(c + 1) * 128], k.ident[:],
                     reads=[b_xt, k.b_const], writes=bPAh[hb])
            k.act(hT[:, c, :], pbank, AF.Identity, reads=bPAh[hb] + [b_sc1, b_cols], writes=[b_hT[c]],
                  scale=sc1[:, c:c + 1], bias=cols[:, c, 0:1])
            if moe and "H" not in _DBG:
                k.act(hT32[:, c, :], pbank, AF.Identity, reads=bPAh[hb] + [b_sc1, b_cols], writes=[b_hT32[c]],
                      scale=sc1[:, c:c + 1], bias=cols[:, c, 0:1])
        if moe and "R" in _DBG:
            for s in range(NSUB):
                k.dve(lambda e, s=s: e.memset(gates[:, s, :], 0.125), reads=[], writes=[b_gates[s]])
        elif moe:
            for s in range(NSUB):
                z = s % 2
                R = rt[z]
                bR = b_rt[z]
                lg, mx8, m1k, m2k, dd, p1, p2 = (R[:, 0:8], R[:, 8:16], R[:, 16:24], R[:, 24:32], R[:, 32:33],
                                                 R[:, 33:34], R[:, 34:35])
                pl = PD[:, s * 8:(s + 1) * 8]
                if "M" in _DBG:
                    k.dve(lambda e, lg=lg, s=s: e.tensor_copy(lg, hT32[:, 0, s * 8:(s + 1) * 8]), reads=[b_hT32[0]], writes=[bR])
                else:
                    for kk in range(8):
                        k.mm(pl, hT32[:, kk, s * 128:(s + 1) * 128], wr[:, kk, :], kk == 0, kk == 7,
                             reads=[b_hT32[kk], b_wr], writes=[bPD[0]])
                    k.dve(lambda e, lg=lg, pl=pl: e.tensor_copy(lg, pl), reads=[bPD[0]], writes=[bR])
                k.dve(lambda e, lg=lg, mx8=mx8: e.max(mx8, lg), reads=[bR], writes=[bR])
                k.dve(lambda e, lg=lg, mx8=mx8, m1k=m1k: e.tensor_scalar(m1k, lg, mx8[:, 0:1], None, op0=ALU.is_equal),
                      reads=[bR], writes=[bR])
                k.dve(lambda e, lg=lg, mx8=mx8, m2k=m2k: e.tensor_scalar(m2k, lg, mx8[:, 1:2], None, op0=ALU.is_equal),
                      reads=[bR], writes=[bR])
                k.dve(lambda e, mx8=mx8, dd=dd: e.tensor_tensor(dd, mx8[:, 1:2], mx8[:, 0:1], op=ALU.subtract),
                      reads=[bR], writes=[bR])
                k.act(p2, dd, AF.Exp, reads=[bR], writes=[bR])
                k.dve(lambda e, p1=p1, p2=p2: e.tensor_scalar_add(p1, p2, 1.0), reads=[bR], writes=[bR])
                k.dve(lambda e, p1=p1: e.reciprocal(p1, p1), reads=[bR], writes=[bR])
                k.dve(lambda e, p1=p1, p2=p2: e.tensor_tensor(p2, p2, p1, op=ALU.mult), reads=[bR], writes=[bR])
                k.dve(lambda e, s=s, m1k=m1k, p1=p1: e.tensor_scalar_mul(gates[:, s, :], m1k, p1),
                      reads=[bR], writes=[b_gates[s]])
                k.dve(lambda e, s=s, m2k=m2k, p2=p2: e.scalar_tensor_tensor(
                    gates[:, s, :], in0=m2k, scalar=p2, in1=gates[:, s, :], op0=ALU.mult, op1=ALU.add),
                    reads=[bR, b_gates[s]], writes=[b_gates[s]])
        for ei, We in enumerate(experts):
            for gi in range(14):
                i = st["w13"] % 2
                st["w13"] += 1
                k.dma("sp", w13s[i], We["w13"][gi].rearrange("p (k i n) -> p k i n", k=8, i=2),
                      reads=[We["b_w13"][gi]], writes=[b_w13s[i]])
                for i2 in range(2):
                    f = gi * 2 + i2
                    pa, pba = next_bank()
                    for kk in range(8):
                        k.mm(pa, w13s[i][:, kk, 0, i2 * 128:(i2 + 1) * 128], hT[:, kk, :], kk == 0, kk == 7,
                             reads=[b_w13s[i], b_hT[kk]], writes=[pba])
                    pb_, pbb = next_bank()
                    for kk in range(8):
                        k.mm(pb_, w13s[i][:, kk, 1, i2 * 128:(i2 + 1) * 128], hT[:, kk, :], kk == 0, kk == 7,
                             reads=[b_w13s[i], b_hT[kk]], writes=[pbb])
                    u = st["u"] % 2
                    st["u"] += 1
                    k.act(sa[u], pa, AF.Silu, reads=[pba], writes=[b_sa[u]])
                    k.dve(lambda e, f=f, u=u, pb_=pb_: e.tensor_tensor(g[:, f, :], sa[u], pb_, op=ALU.mult),
                          reads=[b_sa[u], pbb], writes=[b_g[f]])
            for n in range(2):
                i = st["w2"] % 2
                st["w2"] += 1
                k.dma("sp", w2s[i], We["w2"][n].rearrange("p (f c) -> p f c", f=NF),
                      reads=[We["b_w2"][n]], writes=[b_w2s[i]])
                for s in range(NSUB):
                    py, pby = next_bank()
                    for f in range(NF):
                        k.mm(py, g[:, f, s * 128:(s + 1) * 128], w2s[i][:, f, :], f == 0, f == NF - 1,
                             reads=[b_g[f], b_w2s[i]], writes=[pby])
                    ydst = yacc[:, s, n * 512:(n + 1) * 512]
                    if not moe:
                        k.dve(lambda e, ydst=ydst, py=py, n=n: e.tensor_tensor(ydst, py, G2[:, n * 512:(n + 1) * 512],
                                                                               op=ALU.mult),
                              reads=[pby, b_G2], writes=[b_y[s]])
                    elif ei == 0:
                        k.dve(lambda e, ydst=ydst, py=py, s=s, ei=ei: e.tensor_scalar_mul(ydst, py, gates[:, s, ei:ei + 1]),
                              reads=[pby, b_gates[s]], writes=[b_y[s]])
                    else:
                        k.dve(lambda e, ydst=ydst, py=py, s=s, ei=ei: e.scalar_tensor_tensor(
                            ydst, in0=py, scalar=gates[:, s, ei:ei + 1], in1=ydst, op0=ALU.mult, op1=ALU.add),
                            reads=[pby, b_gates[s], b_y[s]], writes=[b_y[s]])
        for s in range(NSUB):
            z = s % 2
            if moe:
                k.pool(lambda e, s=s: e.tensor_tensor(yacc[:, s, :], yacc[:, s, :], G2, op=ALU.mult),
                       reads=[b_y[s], b_G2], writes=[b_y[s]])
            o = ln_epilogue(k, None, None, yacc[:, s, :], b_y[s], xt[:, s, :], b_xt, None, None, lng, b_lng, lnb, b_lnb,
                            sm[z], b_sm[z], x_dst[t0 + s * 128:t0 + (s + 1) * 128, :])
    P.barrier()


NCORES = 8
NT_CORE = 4096
DEPTH = 4
FUSED = True
_PROGS = {}


def _prm_inputs(k, sub):
    p = {"ln_g": k.din("ln_g", [1, D]), "ln_b": k.din("ln_b", [1, D])}
    if sub == 0:
        p.update({"b_gates": k.din("b_gates", [1, 8]), "conv_qk": k.din("conv_qk", [4, 2 * D]),
                  "hn_gain": k.din("hn_gain", [1, D]), "conv_short": k.din("conv_short", [3, D])})
    return p


def build_p0():
    k = K(NT_CORE)
    c_d = k.din("c", [1, D])
    w_ada = k.din("w_ada", [DEPTH, D, 6 * D])
    b_ada = k.din("b_ada", [DEPTH, 6 * D])
    modd = k.dout("modd", [DEPTH, 6 * D])
    k.P.barrier()
    phase_p0(k, c_d, w_ada, b_ada, modd, DEPTH)
    return k.finish()


def build_mix():
    k = K(NT_CORE)
    x_d = k.din("x", [NT_CORE, D])
    modd = k.din("modd", [1, 6 * D])
    w_in = k.din("w_in", [D, PW])
    w_out = k.din("w_out", [D, D])
    prm = _prm_inputs(k, 0)
    xfer_in = k.din("xfer_in", [128, XF])
    flag_d = k.din("flag", [128, 1])
    x_o = k.dout("x_out", [NT_CORE, D])
    xfer_out = k.dout("xfer_out", [128, XF])
    k.load_flag(flag_d)
    k.P.barrier()
    W = conv_mixer_weights(k, "m", w_in, w_out)
    k.flush()
    phase_mixer(k, x_d, x_o, modd, prm, W, xfer_in, xfer_out)
    return k.finish()


def build_ffn(moe):
    k = K(NT_CORE)
    x_d = k.din("x", [NT_CORE, D])
    modd = k.din("modd", [1, 6 * D])
    prm = _prm_inputs(k, 1)
    x_o = k.dout("x_out", [NT_CORE, D])
    k.P.barrier()
    if moe:
        w13 = k.din("w13", [NE, D, 2 * DFF])
        w2 = k.din("w2", [NE, DFF, D])
        wr = k.din("wr", [D, NE])
        Ws = [conv_ffn_weights(k, f"e{e}", w13[e], w2[e]) for e in range(NE)]
        k.flush()
        phase_ffn(k, x_d, x_o, modd, prm, Ws, wr)
    else:
        w13 = k.din("w13", [D, 2 * DFF])
        w2 = k.din("w2", [DFF, D])
        Ws = [conv_ffn_weights(k, "d", w13, w2)]
        k.flush()
        phase_ffn(k, x_d, x_o, modd, prm, Ws, None)
    return k.finish()


def _prog(name):
    if name not in _PROGS:
        _PROGS[name] = {"p0": build_p0, "mix": build_mix, "ffn_d": lambda: build_ffn(False),
                        "ffn_m": lambda: build_ffn(True)}[name]()
    return _PROGS[name]


def _run(nc, in_maps):
    res = run_bass_kernel_spmd(nc, in_maps, core_ids=list(range(NCORES)))
    return res.results


def kernel_unfused(inp):
    f32 = lambda a: np.ascontiguousarray(np.asarray(a, dtype=np.float32))
    x = f32(inp["x"])
    B, S, _ = x.shape
    half = S // 2
    xs = [x[c // 2, (c % 2) * half:(c % 2 + 1) * half] for c in range(NCORES)]
    cc = f32(inp["c"])
    w_ada, b_ada = f32(inp["w_ada"]), f32(inp["b_ada"])
    r = _run(_prog("p0"), [{"c": cc[c // 2:c // 2 + 1], "w_ada": w_ada, "b_ada": b_ada} for c in range(NCORES)])
    modd = [r[c]["modd"] for c in range(NCORES)]
    zer = np.zeros((128, XF), np.float32)
    for l in range(DEPTH):
        base = {"w_in": f32(inp["w_in"][l]), "w_out": f32(inp["w_out"][l]), "b_gates": f32(inp["b_gates"][l])[None],
                "conv_qk": f32(inp["conv_qk"][l]), "hn_gain": f32(inp["hn_gain"][l])[None],
                "conv_short": f32(inp["conv_short"][l]), "ln_g": f32(inp["ln_g"][l, 0])[None],
                "ln_b": f32(inp["ln_b"][l, 0])[None]}
        xfer = [zer] * NCORES
        for p in range(2):
            maps = [dict(base, x=xs[c], modd=modd[c][l:l + 1], xfer_in=xfer[c],
                         flag=np.full((128, 1), float(c % 2) if p == 1 else 0.0, np.float32)) for c in range(NCORES)]
            r = _run(_prog("mix"), maps)
            xfer = [r[c & ~1]["xfer_out"] for c in range(NCORES)]
        xm = [r[c]["x_out"] for c in range(NCORES)]
        fb = {"ln_g": f32(inp["ln_g"][l, 1])[None], "ln_b": f32(inp["ln_b"][l, 1])[None]}
        if l % 2 == 0:
            fb.update(w13=f32(inp["dense_w13"][l // 2]), w2=f32(inp["dense_w2"][l // 2]))
            name = "ffn_d"
        else:
            fb.update(w13=f32(inp["moe_w13"][l // 2]), w2=f32(inp["moe_w2"][l // 2]), wr=f32(inp["w_router"][l // 2]))
            name = "ffn_m"
        r = _run(_prog(name), [dict(fb, x=xm[c], modd=modd[c][l:l + 1]) for c in range(NCORES)])
        xs = [r[c]["x_out"] for c in range(NCORES)]
    out = np.empty((B, S, D), np.float32)
    for c in range(NCORES):
        out[c // 2, (c % 2) * half:(c % 2 + 1) * half] = xs[c]
    return out


def build_fused():
    k = K(NT_CORE)
    x_d = k.din("x", [NT_CORE, D])
    c_d = k.din("c", [1, D])
    flag_d = k.din("flag", [128, 1])
    w_ada = k.din("w_ada", [DEPTH, D, 6 * D])
    b_ada = k.din("b_ada", [DEPTH, 6 * D])
    w_in = k.din("w_in", [DEPTH, D, PW])
    b_gates = k.din("b_gates", [DEPTH, 8])
    conv_qk = k.din("conv_qk", [DEPTH, 4, 2 * D])
    hn_gain = k.din("hn_gain", [DEPTH, D])
    conv_short = k.din("conv_short", [DEPTH, 3, D])
    w_out = k.din("w_out", [DEPTH, D, D])
    ln_g = k.din("ln_g", [DEPTH, 2, D])
    ln_b = k.din("ln_b", [DEPTH, 2, D])
    dense_w13 = k.din("dense_w13", [DEPTH // 2, D, 2 * DFF])
    dense_w2 = k.din("dense_w2", [DEPTH // 2, DFF, D])
    w_router = k.din("w_router", [DEPTH // 2, D, NE])
    moe_w13 = k.din("moe_w13", [DEPTH // 2, NE, D, 2 * DFF])
    moe_w2 = k.din("moe_w2", [DEPTH // 2, NE, DFF, D])
    out_d = k.dout("out", [NT_CORE, D])
    modd = k.dint("modd", [DEPTH, 6 * D])
    xm = k.dint("xm", [NT_CORE, D])
    xa = k.dint("xa", [NT_CORE, D])
    send = k.dint("xsend", [128, XF])
    gath = k.dint("xgath", [256, XF])
    b_send, b_gath = Buf("send"), Buf("gath")
    k.load_flag(flag_d)
    k.P.barrier()
    phase_p0(k, c_d, w_ada, b_ada, modd, DEPTH)
    Wm = conv_mixer_weights(k, "m0", w_in[0], w_out[0])
    k.flush()
    x_cur = x_d
    for l in range(DEPTH):
        prm0 = {"b_gates": b_gates[l:l + 1, :], "conv_qk": conv_qk[l], "hn_gain": hn_gain[l:l + 1, :],
                "conv_short": conv_short[l], "ln_g": ln_g[l, 0:1, :], "ln_b": ln_b[l, 0:1, :]}
        prm1 = {"ln_g": ln_g[l, 1:2, :], "ln_b": ln_b[l, 1:2, :]}
        if l % 2 == 0:
            Wf = [conv_ffn_weights(k, f"d{l}", dense_w13[l // 2], dense_w2[l // 2])]
            router = None
        else:
            Wf = [conv_ffn_weights(k, f"e{l}_{e}", moe_w13[l // 2, e], moe_w2[l // 2, e]) for e in range(NE)]
            router = w_router[l // 2]
        k.pump_rate = -(-len(k.deferred) // (2 * k.NTILE))
        phase_mixer(k, x_cur, xm, modd[l:l + 1, :], prm0, Wm, None, send, b_out=b_send, prepass=True)
        k.P.op("cc", lambda e: e.collective_compute("AllGather", ALU.bypass,
                                                   replica_groups=[[0, 1], [2, 3], [4, 5], [6, 7]],
                                                   ins=[send.opt()], outs=[gath.opt()]),
               reads=[b_send], writes=[b_gath])
        phase_mixer(k, x_cur, xm, modd[l:l + 1, :], prm0, Wm, gath[0:128, :], None, b_in=b_gath)
        k.flush()
        if l + 1 < DEPTH:
            Wm = conv_mixer_weights(k, f"m{l + 1}", w_in[l + 1], w_out[l + 1])
            k.pump_rate = -(-len(k.deferred) // k.NTILE)
        dst = out_d if l == DEPTH - 1 else xa
        phase_ffn(k, xm, dst, modd[l:l + 1, :], prm1, Wf, router)
        k.flush()
        x_cur = xa
    return k.finish()


def kernel_fused(inp):
    f32 = lambda a: np.ascontiguousarray(np.asarray(a, dtype=np.float32))
    x = f32(inp["x"])
    B, S, _ = x.shape
    half = S // 2
    if "fused" not in _PROGS:
        _PROGS["fused"] = build_fused()
    shared = {n: f32(inp[n]) for n in ("w_ada", "b_ada", "w_in", "b_gates", "conv_qk", "hn_gain", "conv_short", "w_out",
                                       "ln_g", "ln_b", "dense_w13", "dense_w2", "w_router", "moe_w13", "moe_w2")}
    cc = f32(inp["c"])
    maps = []
    for c in range(NCORES):
        m = dict(shared)
        m["x"] = x[c // 2, (c % 2) * half:(c % 2 + 1) * half]
        m["c"] = cc[c // 2:c // 2 + 1]
        m["flag"] = np.full((128, 1), float(c % 2), np.float32)
        maps.append(m)
    r = _run(_PROGS["fused"], maps)
    out = np.empty((B, S, D), np.float32)
    for c in range(NCORES):
        out[c // 2, (c % 2) * half:(c % 2 + 1) * half] = r[c]["out"]
    return out


def kernel(**inputs):
    if FUSED:
        return kernel_fused(inputs)
    return kernel_unfused(inputs)
```
